# Optimizing a Trainium2 kernel written in Bass

```python
import jax, jax.numpy as jnp
from jax import lax
import numpy as np

D_MODEL = 2048
BATCH = 1
SEQ = 8192
DEPTH = 1

GMLP_WIDTH = 1024
GMLP_GROUPS = 8
GMLP_GROUP_DIM = GMLP_WIDTH // GMLP_GROUPS
CHUNK = 128
ATTN_HEADS = 8
HEAD_DIM = 128
ATTN_WIDTH = ATTN_HEADS * HEAD_DIM
Q_LORA_RANK = 512
IDX_HEADS = 16
IDX_HEAD_DIM = 64
IDX_ROPE_DIM = 32
INDEX_TOPK = 256
QUERY_BLOCK = 128
ROPE_THETA = 10000.0
FFN_HIDDEN = -(-8 * D_MODEL // (3 * 256)) * 256
N_MOD = 6
NORM_EPS = 1e-6

COL_UV = 0
COL_QLAT = COL_UV + 2 * GMLP_WIDTH
COL_K = COL_QLAT + Q_LORA_RANK
COL_V = COL_K + ATTN_WIDTH
COL_KIDX = COL_V + ATTN_WIDTH
COL_IDXW = COL_KIDX + IDX_HEAD_DIM
COL_GATE = COL_IDXW + IDX_HEADS
IN_COLS = COL_GATE + 2 * D_MODEL

kernel_name = "hybrid_gmlp_dsa_gated_block"


def rms_norm(x, g):
    xf = x.astype(jnp.float32)
    y = xf * lax.rsqrt(jnp.mean(xf * xf, axis=-1, keepdims=True) + NORM_EPS)
    return (y * g.astype(jnp.float32)).astype(x.dtype)


def layer_norm(x, g, b):
    xf = x.astype(jnp.float32)
    mu = jnp.mean(xf, axis=-1, keepdims=True)
    xc = xf - mu
    var = jnp.mean(xc * xc, axis=-1, keepdims=True)
    return (xc * lax.rsqrt(var + NORM_EPS) * g.astype(jnp.float32) + b.astype(jnp.float32)).astype(x.dtype)


def rope_tables(seq, dim):
    inv = 1.0 / (ROPE_THETA ** (jnp.arange(0, dim, 2, dtype=jnp.float32) / dim))
    ang = jnp.arange(seq, dtype=jnp.float32)[:, None] * inv[None, :]
    return jnp.cos(ang), jnp.sin(ang)


def apply_rope(x, cos, sin):
    xf = x.astype(jnp.float32)
    half = xf.shape[-1] // 2
    x1, x2 = xf[..., :half], xf[..., half:]
    c = cos[None, :, None, :]
    s = sin[None, :, None, :]
    return jnp.concatenate([x1 * c - x2 * s, x2 * c + x1 * s], axis=-1).astype(x.dtype)


def apply_partial_rope(x, cos, sin):
    return jnp.concatenate([apply_rope(x[..., :IDX_ROPE_DIM], cos, sin), x[..., IDX_ROPE_DIM:]], axis=-1)


def gmlp_spatial_gating(uv, ln_g, ln_b, w_s, b_s):
    B, S, _ = uv.shape
    z = jax.nn.gelu(uv)
    u, v = z[..., :GMLP_WIDTH], z[..., GMLP_WIDTH:]
    v = layer_norm(v, ln_g, ln_b)
    v = v.reshape(B, S // CHUNK, CHUNK, GMLP_GROUPS, GMLP_GROUP_DIM)
    causal = jnp.tril(jnp.ones((CHUNK, CHUNK), dtype=bool))
    w = jnp.where(causal[None], w_s, 0).astype(v.dtype)
    sv = jnp.einsum('gts,bcsgd->bctgd', w, v) + b_s.T.astype(v.dtype)[None, None, :, :, None]
    return u * sv.reshape(B, S, GMLP_WIDTH)


def dsa_attention(q, k, v, q_idx, k_idx, idx_w):
    B, S, H, Dh = q.shape
    topk = min(INDEX_TOPK, S // 4)
    n_blocks = S // QUERY_BLOCK
    key_pos = jnp.arange(S)
    scale = HEAD_DIM ** -0.5
    gather = jax.vmap(lambda kk, ii: kk[ii])

    def one_block(i):
        t0 = i * QUERY_BLOCK
        qb = lax.dynamic_slice_in_dim(q, t0, QUERY_BLOCK, axis=1)
        qib = lax.dynamic_slice_in_dim(q_idx, t0, QUERY_BLOCK, axis=1)
        wb = lax.dynamic_slice_in_dim(idx_w, t0, QUERY_BLOCK, axis=1)
        q_pos = t0 + jnp.arange(QUERY_BLOCK)
        causal = key_pos[None, :] <= q_pos[:, None]
        logits = jnp.einsum('bqhd,bsd->bqhs', qib, k_idx).astype(jnp.float32)
        score = jnp.einsum('bqhs,bqh->bqs', jax.nn.relu(logits), wb.astype(jnp.float32))
        score = jnp.where(causal[None], score, -jnp.inf)
        _, sel = lax.top_k(score, topk)
        valid = sel <= q_pos[None, :, None]
        k_sel = gather(k, sel)
        v_sel = gather(v, sel)
        att = jnp.einsum('bqhd,bqkhd->bhqk', qb, k_sel).astype(jnp.float32) * scale
        att = jnp.where(valid[:, None], att, -jnp.inf)
        p = jax.nn.softmax(att, axis=-1).astype(v.dtype)
        return jnp.einsum('bhqk,bqkhd->bqhd', p, v_sel)

    out = lax.map(one_block, jnp.arange(n_blocks))
    return out.transpose(1, 0, 2, 3, 4).reshape(B, S, H * Dh)


def setup_inputs(seed: int = 0) -> dict:
    key = jax.random.key(seed)
    ks = iter(jax.random.split(key, 32))
    f32 = jnp.float32

    def nrm(shape, scale):
        return jax.random.normal(next(ks), shape, f32) * scale

    def gain(shape):
        return 1.0 + 0.1 * jax.random.normal(next(ks), shape, f32)

    L = DEPTH
    return {
        "x": nrm((BATCH, SEQ, D_MODEL), 1.0),
        "c": nrm((BATCH, D_MODEL), 1.0),
        "w_mod": nrm((L, D_MODEL, N_MOD * D_MODEL), 0.5 * D_MODEL ** -0.5),
        "b_mod": nrm((L, N_MOD * D_MODEL), 0.01),
        "g_pre_mix": gain((L, D_MODEL)),
        "g_post_mix": gain((L, D_MODEL)),
        "w_in": nrm((L, D_MODEL, IN_COLS), D_MODEL ** -0.5),
        "gmlp_ln_g": gain((L, GMLP_WIDTH)),
        "gmlp_ln_b": nrm((L, GMLP_WIDTH), 0.02),
        "gmlp_w_s": nrm((L, GMLP_GROUPS, CHUNK, CHUNK), CHUNK ** -0.5),
        "gmlp_b_s": gain((L, GMLP_GROUPS, CHUNK)),
        "q_lat_norm_g": gain((L, Q_LORA_RANK)),
        "w_q_up": nrm((L, Q_LORA_RANK, ATTN_WIDTH), Q_LORA_RANK ** -0.5),
        "w_qidx_up": nrm((L, Q_LORA_RANK, IDX_HEADS * IDX_HEAD_DIM), Q_LORA_RANK ** -0.5),
        "kidx_ln_g": gain((L, IDX_HEAD_DIM)),
        "kidx_ln_b": nrm((L, IDX_HEAD_DIM), 0.02),
        "w_proj_a": nrm((L, GMLP_WIDTH, D_MODEL), GMLP_WIDTH ** -0.5),
        "w_proj_b": nrm((L, ATTN_WIDTH, D_MODEL), ATTN_WIDTH ** -0.5),
        "w_out": nrm((L, D_MODEL, D_MODEL), D_MODEL ** -0.5),
        "g_pre_ffn": gain((L, D_MODEL)),
        "g_post_ffn": gain((L, D_MODEL)),
        "w_ffn_gate": nrm((L, D_MODEL, FFN_HIDDEN), D_MODEL ** -0.5),
        "w_ffn_up": nrm((L, D_MODEL, FFN_HIDDEN), D_MODEL ** -0.5),
        "w_ffn_down": nrm((L, FFN_HIDDEN, D_MODEL), FFN_HIDDEN ** -0.5),
    }


def reference(x, c, w_mod, b_mod, g_pre_mix, g_post_mix, w_in, gmlp_ln_g, gmlp_ln_b, gmlp_w_s, gmlp_b_s,
              q_lat_norm_g, w_q_up, w_qidx_up, kidx_ln_g, kidx_ln_b, w_proj_a, w_proj_b, w_out,
              g_pre_ffn, g_post_ffn, w_ffn_gate, w_ffn_up, w_ffn_down):
    B, S, D = x.shape
    cos_a, sin_a = rope_tables(S, HEAD_DIM)
    cos_i, sin_i = rope_tables(S, IDX_ROPE_DIM)
    idx_w_scale = (IDX_HEADS ** -0.5) * (IDX_HEAD_DIM ** -0.5)

    for l in range(DEPTH):
        mod = (c @ w_mod[l] + b_mod[l]).reshape(B, N_MOD, D)
        shift_m, scale_m, gate_m = mod[:, 0, None], mod[:, 1, None], mod[:, 2, None]
        shift_f, scale_f, gate_f = mod[:, 3, None], mod[:, 4, None], mod[:, 5, None]

        h = rms_norm(x, g_pre_mix[l]) * (1 + scale_m) + shift_m
        proj = h @ w_in[l]

        y_a = gmlp_spatial_gating(proj[..., COL_UV:COL_QLAT], gmlp_ln_g[l], gmlp_ln_b[l],
                                  gmlp_w_s[l], gmlp_b_s[l])

        q_lat = rms_norm(proj[..., COL_QLAT:COL_K], q_lat_norm_g[l])
        q = apply_rope((q_lat @ w_q_up[l]).reshape(B, S, ATTN_HEADS, HEAD_DIM), cos_a, sin_a)
        k = apply_rope(proj[..., COL_K:COL_V].reshape(B, S, ATTN_HEADS, HEAD_DIM), cos_a, sin_a)
        v = proj[..., COL_V:COL_KIDX].reshape(B, S, ATTN_HEADS, HEAD_DIM)
        q_idx = apply_partial_rope((q_lat @ w_qidx_up[l]).reshape(B, S, IDX_HEADS, IDX_HEAD_DIM), cos_i, sin_i)
        k_idx = layer_norm(proj[..., COL_KIDX:COL_IDXW], kidx_ln_g[l], kidx_ln_b[l])
        k_idx = apply_partial_rope(k_idx[:, :, None, :], cos_i, sin_i)[:, :, 0, :]
        idx_w = proj[..., COL_IDXW:COL_GATE] * idx_w_scale
        y_b = dsa_attention(q, k, v, q_idx, k_idx, idx_w)

        gates = jax.nn.sigmoid(proj[..., COL_GATE:])
        merged = gates[..., :D] * (y_a @ w_proj_a[l]) + gates[..., D:] * (y_b @ w_proj_b[l])
        mix_out = merged @ w_out[l]
        x = x + gate_m * rms_norm(mix_out, g_post_mix[l])

        h2 = rms_norm(x, g_pre_ffn[l]) * (1 + scale_f) + shift_f
        f = (jax.nn.silu(h2 @ w_ffn_gate[l]) * (h2 @ w_ffn_up[l])) @ w_ffn_down[l]
        x = x + gate_f * rms_norm(f, g_post_ffn[l])

    return x
```

```python
import numpy as np
import ml_dtypes
import concourse.bass as bass
import concourse.mybir as mybir
from concourse.bass_utils import run_bass_kernel_spmd

F32 = mybir.dt.float32
BF16 = mybir.dt.bfloat16
U32 = mybir.dt.uint32
AF = mybir.ActivationFunctionType
ALU = mybir.AluOpType
AX = mybir.AxisListType

NC = 8
P = 128
TB = 8
TOK = 1024
D = 2048
HID = 5632
NIT = 20
COL_QLAT, COL_K, COL_V, COL_KIDX, COL_IDXW, COL_GATE = 2048, 2560, 3584, 4608, 4672, 4688
IN_COLS = 8784
EPS = 1e-6
NEG = -1.0e30
IDXW_SCALE = (16 ** -0.5) * (64 ** -0.5)
ATT_SCALE = 128 ** -0.5


def core_blocks(c):
    return sorted([16 * j + c for j in range(4)] + [16 * j + 15 - c for j in range(4)])


class _Op:
    __slots__ = ("eng", "fn", "deps", "kind", "semkey", "token", "need", "eidx", "inc")


class Sched:
    def __init__(self, nc):
        self.nc = nc
        self.engs = {"pe": nc.tensor, "act": nc.scalar, "dve": nc.vector, "pool": nc.gpsimd, "sp": nc.sync}
        self.esem = {e: nc.alloc_semaphore("es_" + e) for e in self.engs}
        self.ecount = {e: 0 for e in self.engs}
        self.dsem = {}
        self.dcount = {}
        self.dlast = {}
        self.ncc = 0
        self._reset()

    def _reset(self):
        self.ops = []
        self.lastw = {}
        self.readers = {}
        self.eops = {e: 0 for e in self.engs}

    def add(self, eng, fn, reads=(), writes=(), kind="c", semkey=None):
        op = _Op()
        op.eng, op.fn, op.kind, op.semkey = eng, fn, kind, semkey
        op.need = False
        op.token = None
        deps = []
        for r in reads:
            w = self.lastw.get(r)
            if w is not None:
                deps.append(w)
        for r in writes:
            w = self.lastw.get(r)
            if w is not None:
                deps.append(w)
            for rd in self.readers.get(r, {}).values():
                deps.append(rd)
        if kind == "dma":
            prev = self.dlast.get(semkey)
            if prev is not None:
                deps.append(prev)
            self.dlast[semkey] = op
        op.deps = deps
        rk = eng if kind == "c" else (kind, semkey, id(op))
        for r in reads:
            self.readers.setdefault(r, {})[rk] = op
        for r in writes:
            self.lastw[r] = op
            self.readers[r] = {}
        op.eidx = self.eops[eng]
        self.eops[eng] += 1
        self.ops.append(op)
        return op

    def dma(self, q, out, in_, reads, writes, semkey):
        return self.add(q, lambda e, o=out, i=in_: e.dma_start(out=o, in_=i), reads, writes, "dma", semkey)

    def emit(self):
        nc = self.nc
        ops = self.ops
        phase_ops = set(id(o) for o in ops)
        for op in ops:
            nd = []
            seen = set()
            for d in op.deps:
                if id(d) in seen or d is op:
                    continue
                seen.add(id(d))
                if id(d) not in phase_ops:
                    continue
                if d.kind == "c" and d.eng == op.eng:
                    if op.eng == "pe":
                        continue
                    if op.eidx - d.eidx > 2:
                        continue
                nd.append(d)
                d.need = True
            op.deps = nd
        for op in ops:
            if op.kind == "dma":
                if op.semkey not in self.dsem:
                    self.dsem[op.semkey] = nc.alloc_semaphore("ds%d" % len(self.dsem))
                    self.dcount[op.semkey] = 0
                self.dcount[op.semkey] += 16
                op.token = (self.dsem[op.semkey], self.dcount[op.semkey])
                op.inc = 16
            elif op.kind == "cc":
                s = nc.alloc_semaphore("cc%d" % self.ncc)
                self.ncc += 1
                op.token = (s, 1)
                op.inc = 1
            elif op.need:
                self.ecount[op.eng] += 1
                op.token = (self.esem[op.eng], self.ecount[op.eng])
                op.inc = 1
        per = {e: [o for o in ops if o.eng == e] for e in self.engs}
        waited = {e: {} for e in self.engs}

        def run(e, lst, ename):
            wd = waited[ename]
            for op in lst:
                for d in op.deps:
                    sem, val = d.token
                    if wd.get(sem.num, 0) >= val:
                        continue
                    wd[sem.num] = val
                    e.wait_ge(sem, val)
                ins = op.fn(e)
                if op.token is not None:
                    ins.then_inc(op.token[0], op.inc)

        with nc.Block() as block:
            if per["pe"]:
                @block.tensor
                def _(e):
                    run(e, per["pe"], "pe")
            if per["act"]:
                @block.scalar
                def _(e):
                    run(e, per["act"], "act")
            if per["dve"]:
                @block.vector
                def _(e):
                    run(e, per["dve"], "dve")
            if per["pool"]:
                @block.gpsimd
                def _(e):
                    run(e, per["pool"], "pool")
            if per["sp"]:
                @block.sync
                def _(e):
                    run(e, per["sp"], "sp")
        self._reset()


DEBUG = False
DBG_NAMES = None
STOP = None
_DBG = {}


def build_program(debug=False):
    nc = bass.Bass("TRN2", target_bir_lowering=False)
    S = Sched(nc)
    dbg_keys = []

    def DUMP(name, src, reads, dt=F32):
        if not debug or (DBG_NAMES is not None and name not in DBG_NAMES):
            return
        t = nc.dram_tensor("dbg_" + name, list(src.shape), dt, kind="ExternalOutput").ap()
        S.dma("sp", t, src, list(reads), ["dbg_" + name], ("dbg", name))
        dbg_keys.append("dbg_" + name)

    def DFENCE():
        if debug and dbg_keys:
            S.add("dve", lambda e: e.memset(st[:, 50:51], 0.0), list(dbg_keys), ["st50"])
            del dbg_keys[:]

    def din(name, shape, dt=F32):
        return nc.dram_tensor(name, list(shape), dt, kind="ExternalInput").ap()

    x_d = din("x", [TOK, D])
    ccol_d = din("c_col", [P, 16])
    wmod_d = din("w_mod_sl", [D, 1536])
    bmod_d = din("b_mod_sl", [1, 1536])
    gpm_d = din("g_pre_mix_col", [P, 16])
    gpf_d = din("g_pre_ffn_col", [P, 16])
    gqm_d = din("g_post_mix", [1, D])
    gqf_d = din("g_post_ffn", [1, D])
    win_d = din("w_in", [D, IN_COLS])
    lng_d = din("gmlp_ln_g", [1, 1024])
    lnb_d = din("gmlp_ln_b", [1, 1024])
    wsT_d = din("gmlp_wsT", [P, 8, P])
    bsc_d = din("gmlp_bs_col", [P, 8])
    qg_d = din("q_lat_g_col", [P, 4])
    wq_d = din("w_q_up", [512, 1024])
    wqi_d = din("w_qidx_up", [512, 1024])
    kig_d = din("kidx_ln_g", [1, 64])
    kib_d = din("kidx_ln_b", [1, 64])
    wpa_d = din("w_proj_a", [1024, D])
    wpb_d = din("w_proj_b", [1024, D])
    wo_d = din("w_out", [D, D])
    wg_d = din("w_ffn_gate", [D, HID])
    wu_d = din("w_ffn_up", [D, HID])
    wd_d = din("w_ffn_down", [HID, D])
    c2_d = din("rope_c2", [P, TB, 128])
    s2_d = din("rope_s2", [P, TB, 128])
    ci2_d = din("rope_ci2", [P, TB, 32])
    si2_d = din("rope_si2", [P, TB, 32])
    madd_d = din("maskadd", [TB, P, 8, P])
    ident_d = din("ident_bf", [P, P], BF16)
    negi_d = din("negi4", [P, 512], BF16)
    causT_d = din("causalT", [P, P])
    pow2_d = din("pow2", [P, NIT + 1])
    out_d = nc.dram_tensor("out", [TOK, D], F32, kind="ExternalOutput").ap()

    def dint(name, shape, dt):
        return nc.dram_tensor(name, list(shape), dt).ap()

    mod_in = dint("mod_in", [1, 1536], F32)
    mod_all = dint("mod_all", [8, 1536], F32)
    kt_in = dint("kt_in", [1024, 1024], BF16)
    kt_all = dint("kt_all", [8 * 1024, 1024], BF16)
    v_in = dint("v_in", [1024, 1032], BF16)
    v_all = dint("v_all", [8 * 1024, 1032], BF16)
    ki_in = dint("ki_in", [64, 1024], BF16)
    ki_all = dint("ki_all", [8 * 64, 1024], BF16)
    hT_sp = dint("hT_sp", [P, 16 * 1024], BF16)
    yaT_sp = dint("yaT_sp", [P, 8 * 1024], BF16)
    ybT_sp = dint("ybT_sp", [P, 8 * 1024], BF16)
    x1_sp = dint("x1_sp", [TOK, D], F32)
    gmb_sp = dint("gmb_sp", [P, D], F32)
    gfb_sp = dint("gfb_sp", [P, D], F32)

    def sb(name, shape, dt):
        return nc.alloc_sbuf_tensor(name, list(shape), dt)

    ident = sb("ident", [P, P], BF16)
    negi4 = sb("negi4s", [P, 512], BF16)
    onesb = sb("onesb", [P, 1], BF16)
    gsm_c = sb("gsm_c", [P, 16], F32)
    shm_c = sb("shm_c", [P, 16], F32)
    gsf_c = sb("gsf_c", [P, 16], F32)
    shf_c = sb("shf_c", [P, 16], F32)
    sgn = sb("sgn", [P, TB, 16], F32)
    wabs = sb("wabs", [P, TB, 16], F32)
    KI = sb("KI", [P, TB, 80], F32)
    st = sb("stats", [P, 64], F32)

    ps = [nc.alloc_psum_tensor("ps%d" % i, [P, 512], F32) for i in range(8)]

    def psbf(i):
        return ps[i][:, :].bitcast(BF16)

    A = S.add
    Q = "sp"

    def act_fn(out, in_, func, **kw):
        return lambda e: e.activation(out=out, in_=in_, func=func, **kw)

    def ts(out, in0, s1, s2, op0, op1=None, **kw):
        if op1 is None:
            return lambda e: e.tensor_scalar(out=out, in0=in0, scalar1=s1, scalar2=None, op0=op0, **kw)
        return lambda e: e.tensor_scalar(out=out, in0=in0, scalar1=s1, scalar2=s2, op0=op0, op1=op1, **kw)

    def tt(out, in0, in1, op):
        return lambda e: e.tensor_tensor(out=out, in0=in0, in1=in1, op=op)

    def stt(out, in0, scalar, in1, op0, op1):
        return lambda e: e.scalar_tensor_tensor(out=out, in0=in0, scalar=scalar, in1=in1, op0=op0, op1=op1)

    def mm(out, lhsT, rhs, start, stop):
        return lambda e: e.matmul(out, lhsT, rhs, start=start, stop=stop)

    def tr(out, in_):
        return lambda e: e.transpose(out, in_, ident[:, :])

    def cp(out, in_):
        return lambda e: e.tensor_copy(out=out, in_=in_)

    def rstd_chain(ssq_ap, n, key):
        t1 = st[:, 60:61]
        t2 = st[:, 61:62]
        r = st[:, 62:63]
        A("dve", ts(t1, ssq_ap, 1.0 / n, EPS, ALU.mult, ALU.add), [key + "_ssq"], ["st60"])
        A("act", act_fn(t2, t1, AF.Sqrt), ["st60"], ["st61"])
        A("dve", lambda e: e.reciprocal(out=r, in_=t2), ["st61"], [key])
        return r

    from contextlib import ExitStack

    def rstd_chain(ssq_ap, ssq_key, n, par=0):
        c0 = 52 + 4 * par
        t1 = st[:, c0:c0 + 1]
        t2 = st[:, c0 + 1:c0 + 2]
        r = st[:, c0 + 2:c0 + 3]
        k = "rs%d_" % par
        A("dve", ts(t1, ssq_ap, 1.0 / n, EPS, ALU.mult, ALU.add), [ssq_key], [k + "a"])
        A("act", act_fn(t2, t1, AF.Sqrt), [k + "a"], [k + "b"])
        A("dve", lambda e: e.reciprocal(out=r, in_=t2), [k + "b"], [k + "r"])
        return r, k + "r"

    def fence(keys):
        A("dve", lambda e: e.memset(st[:, 63:64], 0.0), list(keys), ["st63"])

    _aln = [0]

    def AL(es, name, shape, dt):
        _aln[0] += 1
        return es.enter_context(nc.sbuf_tensor("%s_%d" % (name, _aln[0]), list(shape), dt))

    def bcast_row2(ap2d):
        return ap2d.partition_broadcast(P).rearrange("p o n -> p (o n)")

    with ExitStack() as es:
        wm = AL(es, "wm", [P, 16, 1536], F32)
        ccol = AL(es, "ccol", [P, 16], F32)
        bm = AL(es, "bm", [1, 1536], F32)
        modrow = AL(es, "modrow", [1, 1536], F32)
        gmb = AL(es, "gmb", [P, D], F32)
        gfb = AL(es, "gfb", [P, D], F32)
        gq = AL(es, "gq", [P, D], F32)
        mcols = AL(es, "mcols", [P, 4, 16], F32)
        gpc = AL(es, "gpc", [P, 2, 16], F32)

        S.dma(Q, ident[:, :], ident_d, [], ["ident"], "c0")
        S.dma(Q, negi4[:, :], negi_d, [], ["negi4"], "c1")
        A("dve", lambda e: e.memset(onesb[:, :], 1.0), [], ["onesb"])
        S.dma(Q, ccol[:, :], ccol_d, [], ["ccol"], "c2")
        S.dma(Q, bm[:, :], bmod_d, [], ["bm"], "c3")
        wmv = wmod_d.rearrange("(k p) n -> p k n", p=P)
        for g in range(4):
            S.dma(Q, wm[:, 4 * g:4 * g + 4, :], wmv[:, 4 * g:4 * g + 4, :], [], [("wm", g)], ("wm", g))
        for n in range(3):
            for k in range(16):
                A("pe", mm(ps[n][0:1, :], ccol[:, k:k + 1], wm[:, k, n * 512:(n + 1) * 512], k == 0, k == 15),
                  ["ccol", ("wm", k // 4)], [("ps", n)])
            A("dve", tt(modrow[0:1, n * 512:(n + 1) * 512], ps[n][0:1, :], bm[0:1, n * 512:(n + 1) * 512], ALU.add),
              [("ps", n), "bm"], ["modrow"])
        S.dma(Q, mod_in, modrow[0:1, :], ["modrow"], ["mod_in"], "c4")
        A("pool", lambda e: e.collective_compute("AllGather", ALU.bypass, replica_groups=[list(range(NC))],
                                                 ins=[mod_in], outs=[mod_all]),
          ["mod_in"], ["mod_all"], kind="cc")
        modflat = mod_all.rearrange("a n -> (a n)")

        def mslice(i):
            return modflat[i * D:(i + 1) * D]

        def bcast_row(ap1d):
            return bcast_row2(ap1d.rearrange("(o n) -> o n", o=1))

        S.dma(Q, gmb[:, :], bcast_row(mslice(2)), ["mod_all"], ["gmb"], "c5")
        S.dma(Q, gfb[:, :], bcast_row(mslice(5)), ["mod_all"], ["gfb"], "c6")
        for i, mi in enumerate([0, 1, 3, 4]):
            src = mslice(mi).rearrange("(k p) -> p k", p=P)
            A(Q, lambda e, o=mcols[:, i, :], s=src: e.dma_start(out=o, in_=s, allow_slow_non_contiguous=True),
              ["mod_all"], [("mcols", i)], "dma", ("mc", i))
        S.dma(Q, gpc[:, 0, :], gpm_d, [], ["gpc0"], "c7")
        S.dma(Q, gpc[:, 1, :], gpf_d, [], ["gpc1"], "c8")
        A("dve", stt(gsm_c[:, :], mcols[:, 1, :], 1.0, gpc[:, 0, :], ALU.add, ALU.mult), [("mcols", 1), "gpc0"], ["gsm"])
        A("dve", stt(gsf_c[:, :], mcols[:, 3, :], 1.0, gpc[:, 1, :], ALU.add, ALU.mult), [("mcols", 3), "gpc1"], ["gsf"])
        A("dve", cp(shm_c[:, :], mcols[:, 0, :]), [("mcols", 0)], ["shm"])
        A("dve", cp(shf_c[:, :], mcols[:, 2, :]), [("mcols", 2)], ["shf"])
        S.dma(Q, gq[:, :], bcast_row2(gqm_d), [], ["gq"], "c9")
        A("dve", tt(gmb[:, :], gmb[:, :], gq[:, :], ALU.mult), ["gmb", "gq"], ["gmb"])
        S.dma(Q, gmb_sp, gmb[:, :], ["gmb"], ["gmb_sp"], "c5")
        S.dma(Q, gq[:, :], bcast_row2(gqf_d), ["gq"], ["gq"], "c9")
        A("dve", tt(gfb[:, :], gfb[:, :], gq[:, :], ALU.mult), ["gfb", "gq"], ["gfb"])
        S.dma(Q, gfb_sp, gfb[:, :], ["gfb"], ["gfb_sp"], "c6")
        fence(["gmb_sp", "gfb_sp"])
        DUMP("gsm", gsm_c[:, :], ["gsm"]); DUMP("shm", shm_c[:, :], ["shm"]); DUMP("gsf", gsf_c[:, :], ["gsf"]); DUMP("shf", shf_c[:, :], ["shf"])
        DUMP("gmb", gmb[:, :], ["gmb"]); DUMP("gfb", gfb[:, :], ["gfb"])
        DFENCE()
        S.emit()
        if STOP == "M":
            return nc

    winv = win_d.rearrange("(k p) n -> p k n", p=P)
    esAB = ExitStack()
    qT = AL(esAB, "qT", [P, TB, 8, P], BF16)
    qiT = AL(esAB, "qiT", [P, TB, 8, P], BF16)
    with ExitStack() as esA:
        hT = AL(esA, "hT", [P, 16, TOK], BF16)
        with ExitStack() as es:
            xs = [AL(es, "xs%d" % i, [P, D], F32) for i in range(2)]
            xn = [AL(es, "xn%d" % i, [P, D], BF16) for i in range(2)]
            junk = AL(es, "junk", [P, D], BF16)
            for tb in range(TB):
                b = tb % 2
                S.dma(Q, xs[b][:, :], x_d[tb * P:(tb + 1) * P, :], [], [("xs", b)], ("xs", b))
                ssq = st[:, b:b + 1]
                A("dve", lambda e, o=ssq: e.memset(o, 0.0), [], [("ssq", b)])
                A("act", act_fn(junk[:, :], xs[b][:, :], AF.Square, accum_out=ssq), [("xs", b), ("ssq", b)], ["junk", ("ssq", b)])
                r, rk = rstd_chain(ssq, ("ssq", b), D, b)
                A("dve", ts(xn[b][:, :], xs[b][:, :], r, None, ALU.mult), [("xs", b), rk], [("xn", b)])
                for g4 in range(4):
                    pb = 4 + (tb * 4 + g4) % 4
                    pt = psbf(pb)
                    for i in range(4):
                        k = g4 * 4 + i
                        A("pe", tr(pt[:, i * P:(i + 1) * P], xn[b][:, k * P:(k + 1) * P]), [("xn", b), "ident"], [("ps", pb)])
                    for i in range(4):
                        k = g4 * 4 + i
                        o = hT[:, k, tb * P:(tb + 1) * P]
                        if i % 2 == 0:
                            A("dve", ts(o, pt[:, i * P:(i + 1) * P], gsm_c[:, k:k + 1], shm_c[:, k:k + 1], ALU.mult, ALU.add),
                              [("ps", pb), "gsm", "shm"], [("hT", tb)])
                        else:
                            A("act", act_fn(o, pt[:, i * P:(i + 1) * P], AF.Identity, scale=gsm_c[:, k:k + 1], bias=shm_c[:, k:k + 1]),
                              [("ps", pb), "gsm", "shm"], [("hT", tb)])
            DUMP("hT", hT[:, :, :], [("hT", t_) for t_ in range(TB)], BF16)
            DFENCE()
            S.emit()
            if STOP == "A0":
                return nc

        HT_ALL = [("hT", tb) for tb in range(TB)]
        wcount = [0]

        def load_w(ring, c0, ncol, key):
            i = wcount[0] % len(ring)
            wcount[0] += 1
            S.dma("pool", ring[i][:, :, 0:ncol], winv[:, :, c0:c0 + ncol], [], [(key, i)], (key, i))
            return ring[i], (key, i)

        def proj_tok(wt, wkey, ncol, tb, pbank, col0=0):
            for k in range(16):
                A("pe", mm(ps[pbank][:, 0:ncol], hT[:, k, tb * P:(tb + 1) * P], wt[:, k, col0:col0 + ncol], k == 0, k == 15),
                  [("hT", tb), wkey], [("ps", pbank)])

        with ExitStack() as es:
            ring = [AL(es, "wr%d" % i, [P, 16, 512], BF16) for i in range(2)]
            U = AL(es, "U", [P, TB, 1024], BF16)
            Vr = AL(es, "Vr", [P, TB, 1024], F32)
            x2 = [AL(es, "x2_%d" % i, [P, 512], F32) for i in range(2)]
            inn = [AL(es, "inn%d" % i, [P, 512], F32) for i in range(2)]
            sg = [AL(es, "sg%d" % i, [P, 512], F32) for i in range(2)]
            vhat = [AL(es, "vhat%d" % i, [P, 1024], BF16) for i in range(1)]
            t1 = [AL(es, "t1_%d" % i, [P, 1024], F32) for i in range(1)]
            ya = [AL(es, "ya%d" % i, [P, 1024], BF16) for i in range(1)]
            junk1 = AL(es, "junk1", [P, 1024], BF16)
            wsf = AL(es, "wsf", [P, 8, P], F32)
            WsT = AL(es, "WsT", [P, 8, P], BF16)
            causT = AL(es, "causT", [P, P], F32)
            LG = AL(es, "LG", [P, 1024], F32)
            LB = AL(es, "LB", [P, 1024], F32)
            B0 = AL(es, "B0", [P, 1024], F32)
            bsc = AL(es, "bsc", [P, 8], F32)
            rsum = AL(es, "rsum", [P, 8], F32)
            yaT = AL(es, "yaT", [P, 8, TOK], BF16)

            S.dma(Q, wsf[:, :, :], wsT_d, [], ["wsf"], "c0")
            S.dma(Q, causT[:, :], causT_d, [], ["causT"], "c1")
            S.dma(Q, LG[:, :], bcast_row2(lng_d), [], ["LG"], "c2")
            S.dma(Q, LB[:, :], bcast_row2(lnb_d), [], ["LB"], "c3")
            S.dma(Q, bsc[:, :], bsc_d, [], ["bsc"], "c4")
            A("dve", tt(WsT[:, :, :], wsf[:, :, :], causT[:, :].unsqueeze(1).broadcast_to([P, 8, P]), ALU.mult),
              ["wsf", "causT"], ["WsT"])
            for g in range(8):
                A("pe", mm(ps[7][:, g:g + 1], WsT[:, g, :], onesb[:, 0:1], True, True), ["WsT", "onesb"], [("ps", 7)])
            A("dve", cp(rsum[:, :], ps[7][:, 0:8]), [("ps", 7)], ["rsum"])
            for g in range(8):
                A("dve", ts(B0[:, g * P:(g + 1) * P], LB[:, g * P:(g + 1) * P], rsum[:, g:g + 1], bsc[:, g:g + 1], ALU.mult, ALU.add),
                  ["LB", "rsum", "bsc"], ["B0"])

            def a_branch(tb):
                b = tb % 2
                s1 = st[:, 4 + b:5 + b]
                nm = st[:, 6 + b:7 + b]
                s2 = st[:, 8 + b:9 + b]
                A("dve", lambda e: e.tensor_reduce(out=s1, in_=Vr[:, tb, :], axis=AX.X, op=ALU.add), [("Vr", tb)], [("s1", b)])
                A("dve", ts(nm, s1, -1.0 / 1024, None, ALU.mult), [("s1", b)], [("nm", b)])
                A("dve", ts(Vr[:, tb, :], Vr[:, tb, :], nm, None, ALU.add), [("Vr", tb), ("nm", b)], [("Vr", tb)])
                A("dve", lambda e: e.memset(s2, 0.0), [], [("s2", b)])
                A("act", act_fn(junk1[:, :], Vr[:, tb, :], AF.Square, accum_out=s2), [("Vr", tb), ("s2", b)], ["junk1", ("s2", b)])
                r, rk = rstd_chain(s2, ("s2", b), 1024, b)
                A("dve", ts(vhat[0][:, :], Vr[:, tb, :], r, None, ALU.mult), [("Vr", tb), rk], [("vhat", 0)])
                pv = [5, 6]
                for g in range(8):
                    bank = pv[g // 4]
                    A("pe", mm(ps[bank][:, (g % 4) * P:(g % 4 + 1) * P], WsT[:, g, :], vhat[0][:, g * P:(g + 1) * P], True, True),
                      ["WsT", ("vhat", 0)], [("ps", bank)])
                for hf in range(2):
                    sl = slice(hf * 512, (hf + 1) * 512)
                    A("dve", tt(t1[0][:, sl], ps[pv[hf]][:, :], LG[:, sl], ALU.mult), [("ps", pv[hf]), "LG"], [("t1", 0, hf)])
                    A("dve", tt(t1[0][:, sl], t1[0][:, sl], B0[:, sl], ALU.add), [("t1", 0, hf), "B0"], [("t1", 0, hf)])
                    A("dve", tt(ya[0][:, sl], t1[0][:, sl], U[:, tb, sl], ALU.mult), [("t1", 0, hf), ("U", tb)], [("ya", 0)])
                pt = psbf(7)
                for j in range(8):
                    A("pe", tr(pt[:, j * P:(j + 1) * P], ya[0][:, j * P:(j + 1) * P]), [("ya", 0), "ident"], [("ps", 7)])
                A("act", lambda e: e.activation(out=yaT[:, :, tb * P:(tb + 1) * P], in_=pt.rearrange("p (j t) -> p j t", j=8), func=AF.Copy),
                  [("ps", 7)], ["yaT"])

            cnt = 0
            for ct in range(4):
                wt, wk = load_w(ring, ct * 512, 512, "wr")
                for tb in range(TB):
                    pbk = cnt % 4
                    b = cnt % 2
                    cnt += 1
                    proj_tok(wt, wk, 512, tb, pbk)
                    pp = ps[pbk][:, :]
                    A("act", act_fn(x2[b][:, :], pp, AF.Square), [("ps", pbk)], [("x2", b)])
                    A("dve", stt(inn[b][:, :], x2[b][:, :], 1.0 / 0.044715, pp, ALU.add, ALU.mult), [("x2", b), ("ps", pbk)], [("inn", b)])
                    A("act", act_fn(sg[b][:, :], inn[b][:, :], AF.Sigmoid, scale=1.5957691216 * 0.044715), [("inn", b)], [("sg", b)])
                    if ct < 2:
                        dst, dk = U[:, tb, ct * 512:(ct + 1) * 512], ("U", tb)
                    else:
                        dst, dk = Vr[:, tb, (ct - 2) * 512:(ct - 1) * 512], ("Vr", tb)
                    A("dve", tt(dst, sg[b][:, :], pp, ALU.mult), [("sg", b), ("ps", pbk)], [dk])
                    if ct == 3:
                        a_branch(tb)
            S.dma(Q, yaT_sp, yaT[:, :, :].rearrange("p j t -> p (j t)"), ["yaT"], ["yaT_sp"], "c5")
            fence(["yaT_sp"])
            DUMP("yaT", yaT[:, :, :], ["yaT"], BF16)
            DFENCE()
            S.emit()
            if STOP == "A1":
                return nc

        with ExitStack() as es:
            ring = [AL(es, "wr%d" % i, [P, 16, 512], BF16) for i in range(2)]
            QL = AL(es, "QL", [P, TB, 512], F32)
            qn = [AL(es, "qn%d" % i, [P, 512], BF16) for i in range(2)]
            qlT = AL(es, "qlT", [P, 4, TOK], BF16)
            wq = AL(es, "wq", [P, 4, 1024], BF16)
            wqi = AL(es, "wqi", [P, 4, 1024], BF16)
            C2 = AL(es, "C2", [P, TB, 128], F32)
            S2 = AL(es, "S2", [P, TB, 128], F32)
            Ci2 = AL(es, "Ci2", [P, TB, 32], F32)
            Si2 = AL(es, "Si2", [P, TB, 32], F32)
            qgc = AL(es, "qgc", [P, 4], F32)
            ta = [AL(es, "ta%d" % i, [P, 1024], F32) for i in range(2)]
            tb_ = [AL(es, "tbb%d" % i, [P, 1024], F32) for i in range(2)]
            qr = [AL(es, "qr%d" % i, [P, 1024], BF16) for i in range(2)]
            qi = [AL(es, "qi%d" % i, [P, 16, 64], F32) for i in range(2)]
            ua = [AL(es, "ua%d" % i, [P, 16, 32], F32) for i in range(2)]
            ub = [AL(es, "ub%d" % i, [P, 16, 32], F32) for i in range(2)]
            qib = [AL(es, "qib%d" % i, [P, 16, 64], BF16) for i in range(2)]
            junk2 = AL(es, "junk2", [P, 512], BF16)

            S.dma("pool", wq[:, :, :], wq_d.rearrange("(k p) n -> p k n", p=P), [], ["wq"], "c0")
            S.dma("pool", wqi[:, :, :], wqi_d.rearrange("(k p) n -> p k n", p=P), [], ["wqi"], "c1")
            S.dma(Q, C2[:, :, :], c2_d, [], ["C2"], "c2")
            S.dma(Q, S2[:, :, :], s2_d, [], ["S2"], "c3")
            S.dma(Q, Ci2[:, :, :], ci2_d, [], ["Ci2"], "c4")
            S.dma(Q, Si2[:, :, :], si2_d, [], ["Si2"], "c5")
            S.dma(Q, qgc[:, :], qg_d, [], ["qgc"], "c6")
            wt, wk = load_w(ring, COL_QLAT, 512, "wr")
            for tb in range(TB):
                pbk = tb % 4
                proj_tok(wt, wk, 512, tb, pbk)
                A("dve" if tb % 2 else "act", cp(QL[:, tb, :], ps[pbk][:, :]) if tb % 2 else act_fn(QL[:, tb, :], ps[pbk][:, :], AF.Copy),
                  [("ps", pbk)], [("QL", tb)])
            wt, wk = load_w(ring, COL_KIDX, 80, "wr")
            for tb in range(TB):
                pbk = tb % 4
                proj_tok(wt, wk, 80, tb, pbk)
                A("dve", cp(KI[:, tb, :], ps[pbk][:, 0:80]), [("ps", pbk)], [("KI", tb)])
            for tb in range(TB):
                b = tb % 2
                A("act", act_fn(wabs[:, tb, :], KI[:, tb, 64:80], AF.Abs, scale=IDXW_SCALE), [("KI", tb)], [("wabs", tb)])
                A("act", act_fn(sgn[:, tb, :], KI[:, tb, 64:80], AF.Sign), [("KI", tb)], [("sgn", tb)])
                ssq = st[:, b:b + 1]
                A("dve", lambda e, o=ssq: e.memset(o, 0.0), [], [("ssq", b)])
                A("act", act_fn(junk2[:, :], QL[:, tb, :], AF.Square, accum_out=ssq), [("QL", tb), ("ssq", b)], ["junk2", ("ssq", b)])
                r, rk = rstd_chain(ssq, ("ssq", b), 512, b)
                A("dve", ts(qn[b][:, :], QL[:, tb, :], r, None, ALU.mult), [("QL", tb), rk], [("qn", b)])
                pt = psbf(4 + b)
                for k in range(4):
                    A("pe", tr(pt[:, k * P:(k + 1) * P], qn[b][:, k * P:(k + 1) * P]), [("qn", b), "ident"], [("ps", 4 + b)])
                for k in range(4):
                    A("dve", ts(qlT[:, k, tb * P:(tb + 1) * P], pt[:, k * P:(k + 1) * P], qgc[:, k:k + 1], None, ALU.mult),
                      [("ps", 4 + b), "qgc"], [("qlT", tb)])
                for n in range(2):
                    for k in range(4):
                        A("pe", mm(ps[n][:, :], qlT[:, k, tb * P:(tb + 1) * P], wq[:, k, n * 512:(n + 1) * 512], k == 0, k == 3),
                          [("qlT", tb), "wq"], [("ps", n)])
                for n in range(2):
                    pq = ps[n][:, :].rearrange("p (h f) -> p h f", h=4)
                    tav = ta[b][:, n * 512:(n + 1) * 512].rearrange("p (h f) -> p h f", h=4)
                    tbv = tb_[b][:, n * 512:(n + 1) * 512].rearrange("p (h f) -> p h f", h=4)
                    qrv = qr[b][:, n * 512:(n + 1) * 512].rearrange("p (h f) -> p h f", h=4)
                    c2b = C2[:, tb, :].unsqueeze(1).broadcast_to([P, 4, 128])
                    A("dve", tt(tav, pq, c2b, ALU.mult), [("ps", n), "C2"], [("ta", b, n)])
                    A("dve", tt(tbv[:, :, 0:64], pq[:, :, 64:128], S2[:, tb, 0:64].unsqueeze(1).broadcast_to([P, 4, 64]), ALU.mult),
                      [("ps", n), "S2"], [("tb", b, n)])
                    A("dve", tt(tbv[:, :, 64:128], pq[:, :, 0:64], S2[:, tb, 64:128].unsqueeze(1).broadcast_to([P, 4, 64]), ALU.mult),
                      [("ps", n), "S2"], [("tb", b, n)])
                    A("dve", tt(qrv, tav, tbv, ALU.add), [("ta", b, n), ("tb", b, n)], [("qr", b)])
                pt2 = psbf(6 + b)
                for h in range(8):
                    A("pe", tr(pt2[:, h * P:(h + 1) * P], qr[b][:, h * P:(h + 1) * P]), [("qr", b), "ident"], [("ps", 6 + b)])
                A("act", lambda e, tb=tb, pt2=pt2: e.activation(out=qT[:, tb, :, :], in_=pt2.rearrange("p (h t) -> p h t", h=8), func=AF.Copy),
                  [("ps", 6 + b)], [("qT", tb)])
                for n in range(2):
                    for k in range(4):
                        A("pe", mm(ps[2 + n][:, :], qlT[:, k, tb * P:(tb + 1) * P], wqi[:, k, n * 512:(n + 1) * 512], k == 0, k == 3),
                          [("qlT", tb), "wqi"], [("ps", 2 + n)])
                for n in range(2):
                    pq = ps[2 + n][:, :].rearrange("p (h f) -> p h f", h=8)
                    A("dve", tt(qi[b][:, 8 * n:8 * n + 8, :], pq, wabs[:, tb, 8 * n:8 * n + 8].unsqueeze(2).broadcast_to([P, 8, 64]), ALU.mult),
                      [("ps", 2 + n), ("wabs", tb)], [("qi", b)])
                A("dve", tt(ua[b][:, :, :], qi[b][:, :, 0:32], Ci2[:, tb, :].unsqueeze(1).broadcast_to([P, 16, 32]), ALU.mult),
                  [("qi", b), "Ci2"], [("ua", b)])
                A("dve", tt(ub[b][:, :, 0:16], qi[b][:, :, 16:32], Si2[:, tb, 0:16].unsqueeze(1).broadcast_to([P, 16, 16]), ALU.mult),
                  [("qi", b), "Si2"], [("ub", b)])
                A("dve", tt(ub[b][:, :, 16:32], qi[b][:, :, 0:16], Si2[:, tb, 16:32].unsqueeze(1).broadcast_to([P, 16, 16]), ALU.mult),
                  [("qi", b), "Si2"], [("ub", b)])
                A("dve", tt(qib[b][:, :, 0:32], ua[b][:, :, :], ub[b][:, :, :], ALU.add), [("ua", b), ("ub", b)], [("qib", b)])
                A("dve", cp(qib[b][:, :, 32:64], qi[b][:, :, 32:64]), [("qi", b)], [("qib", b)])
                pt3 = psbf(4 + b)
                qibf = qib[b][:, :, :].rearrange("p h f -> p (h f)")
                for j in range(8):
                    A("pe", tr(pt3[:, j * P:(j + 1) * P], qibf[:, j * P:(j + 1) * P]), [("qib", b), "ident"], [("ps", 4 + b)])
                A("act", lambda e, tb=tb, pt3=pt3: e.activation(out=qiT[:, tb, :, :], in_=pt3.rearrange("p (h t) -> p h t", h=8), func=AF.Copy),
                  [("ps", 4 + b)], [("qiT", tb)])
            DUMP("qT", qT[:, :, :, :], [("qT", t_) for t_ in range(TB)], BF16)
            DUMP("qiT", qiT[:, :, :, :], [("qiT", t_) for t_ in range(TB)], BF16)
            DUMP("sgn", sgn[:, :, :], [("sgn", t_) for t_ in range(TB)])
            DUMP("wabs", wabs[:, :, :], [("wabs", t_) for t_ in range(TB)])
            DFENCE()
            S.emit()
            if STOP == "A2":
                return nc

        with ExitStack() as es:
            ring = [AL(es, "wr%d" % i, [P, 16, 512], BF16) for i in range(3)]
            KTl = AL(es, "KTl", [P, 8, TOK], BF16)
            Vx = AL(es, "Vx", [P, TB, 8, 129], BF16)
            kT64 = AL(es, "kT64", [P, TOK], BF16)
            C2 = AL(es, "C2", [P, TB, 128], F32)
            S2 = AL(es, "S2", [P, TB, 128], F32)
            Ci2 = AL(es, "Ci2", [P, TB, 32], F32)
            Si2 = AL(es, "Si2", [P, TB, 32], F32)
            KG = AL(es, "KG", [P, 64], F32)
            KB = AL(es, "KB", [P, 64], F32)
            ta = [AL(es, "ta%d" % i, [P, 512], F32) for i in range(2)]
            tb_ = [AL(es, "tbb%d" % i, [P, 512], F32) for i in range(2)]
            kr = [AL(es, "kr%d" % i, [P, 512], BF16) for i in range(2)]
            kn = [AL(es, "kn%d" % i, [P, 64], F32) for i in range(2)]
            ka = [AL(es, "ka%d" % i, [P, 32], F32) for i in range(2)]
            kb = [AL(es, "kb%d" % i, [P, 32], F32) for i in range(2)]
            kib = [AL(es, "kib%d" % i, [P, 64], BF16) for i in range(2)]
            junk3 = AL(es, "junk3", [P, 64], BF16)

            S.dma(Q, C2[:, :, :], c2_d, [], ["C2"], "c2")
            S.dma(Q, S2[:, :, :], s2_d, [], ["S2"], "c3")
            S.dma(Q, Ci2[:, :, :], ci2_d, [], ["Ci2"], "c4")
            S.dma(Q, Si2[:, :, :], si2_d, [], ["Si2"], "c5")
            S.dma(Q, KG[:, :], bcast_row2(kig_d), [], ["KG"], "c6")
            S.dma(Q, KB[:, :], bcast_row2(kib_d), [], ["KB"], "c7")
            A("dve", lambda e: e.memset(Vx[:, :, :, 128:129], 1.0), [], ["Vx1"])
            cnt = 0
            wKV = [load_w(ring, COL_K, 512, "wr"), load_w(ring, COL_K + 512, 512, "wr"), load_w(ring, COL_V, 512, "wr")]
            for n in range(2):
                wt, wk = wKV[n]
                for tb in range(TB):
                    pbk = cnt % 4
                    b = cnt % 2
                    cnt += 1
                    proj_tok(wt, wk, 512, tb, pbk)
                    pq = ps[pbk][:, :].rearrange("p (h f) -> p h f", h=4)
                    tav = ta[b][:, :].rearrange("p (h f) -> p h f", h=4)
                    tbv = tb_[b][:, :].rearrange("p (h f) -> p h f", h=4)
                    krv = kr[b][:, :].rearrange("p (h f) -> p h f", h=4)
                    A("dve", tt(tav, pq, C2[:, tb, :].unsqueeze(1).broadcast_to([P, 4, 128]), ALU.mult), [("ps", pbk), "C2"], [("ta", b)])
                    A("dve", tt(tbv[:, :, 0:64], pq[:, :, 64:128], S2[:, tb, 0:64].unsqueeze(1).broadcast_to([P, 4, 64]), ALU.mult),
                      [("ps", pbk), "S2"], [("tb", b)])
                    A("dve", tt(tbv[:, :, 64:128], pq[:, :, 0:64], S2[:, tb, 64:128].unsqueeze(1).broadcast_to([P, 4, 64]), ALU.mult),
                      [("ps", pbk), "S2"], [("tb", b)])
                    A("dve", tt(krv, tav, tbv, ALU.add), [("ta", b), ("tb", b)], [("kr", b)])
                    pt = psbf(4 + b)
                    for h in range(4):
                        A("pe", tr(pt[:, h * P:(h + 1) * P], kr[b][:, h * P:(h + 1) * P]), [("kr", b), "ident"], [("ps", 4 + b)])
                    A("act", lambda e, n=n, tb=tb, pt=pt: e.activation(out=KTl[:, 4 * n:4 * n + 4, tb * P:(tb + 1) * P],
                                                                      in_=pt[:, 0:512].rearrange("p (h t) -> p h t", h=4), func=AF.Copy),
                      [("ps", 4 + b)], ["KTl"])
            S.dma(Q, kt_in.rearrange("(h d) t -> d h t", h=8), KTl[:, :, :], ["KTl"], ["kt_in"], "c8")
            wKV.append(load_w(ring, COL_V + 512, 512, "wr"))
            A("pool", lambda e: e.collective_compute("AllGather", ALU.bypass, replica_groups=[list(range(NC))], ins=[kt_in], outs=[kt_all]),
              ["kt_in"], ["kt_all"], kind="cc")
            for n in range(2):
                wt, wk = wKV[2 + n]
                for tb in range(TB):
                    pbk = cnt % 4
                    cnt += 1
                    proj_tok(wt, wk, 512, tb, pbk)
                    A("act", lambda e, n=n, tb=tb, pbk=pbk: e.activation(out=Vx[:, tb, 4 * n:4 * n + 4, 0:128],
                                                                        in_=ps[pbk][:, :].rearrange("p (h f) -> p h f", h=4), func=AF.Copy),
                      [("ps", pbk)], ["Vx"])
            S.dma(Q, v_in.rearrange("(b s) f -> s b f", b=8), Vx[:, :, :, :].rearrange("p b h f -> p b (h f)"), ["Vx", "Vx1"], ["v_in"], "c9")
            A("pool", lambda e: e.collective_compute("AllGather", ALU.bypass, replica_groups=[list(range(NC))], ins=[v_in], outs=[v_all]),
              ["v_in"], ["v_all"], kind="cc")
            for tb in range(TB):
                b = tb % 2
                s1 = st[:, 4 + b:5 + b]
                nm = st[:, 6 + b:7 + b]
                s2 = st[:, 8 + b:9 + b]
                A("dve", lambda e, s1=s1, tb=tb: e.tensor_reduce(out=s1, in_=KI[:, tb, 0:64], axis=AX.X, op=ALU.add), [("KI", tb)], [("s1", b)])
                A("dve", ts(nm, s1, -1.0 / 64, None, ALU.mult), [("s1", b)], [("nm", b)])
                A("dve", ts(kn[b][:, :], KI[:, tb, 0:64], nm, None, ALU.add), [("KI", tb), ("nm", b)], [("kn", b)])
                A("dve", lambda e, s2=s2: e.memset(s2, 0.0), [], [("s2", b)])
                A("act", act_fn(junk3[:, :], kn[b][:, :], AF.Square, accum_out=s2), [("kn", b), ("s2", b)], ["junk3", ("s2", b)])
                r, rk = rstd_chain(s2, ("s2", b), 64, b)
                A("dve", ts(kn[b][:, :], kn[b][:, :], r, None, ALU.mult), [("kn", b), rk], [("kn", b)])
                A("dve", tt(kn[b][:, :], kn[b][:, :], KG[:, :], ALU.mult), [("kn", b), "KG"], [("kn", b)])
                A("dve", tt(kn[b][:, :], kn[b][:, :], KB[:, :], ALU.add), [("kn", b), "KB"], [("kn", b)])
                A("dve", tt(ka[b][:, :], kn[b][:, 0:32], Ci2[:, tb, :], ALU.mult), [("kn", b), "Ci2"], [("ka", b)])
                A("dve", tt(kb[b][:, 0:16], kn[b][:, 16:32], Si2[:, tb, 0:16], ALU.mult), [("kn", b), "Si2"], [("kb", b)])
                A("dve", tt(kb[b][:, 16:32], kn[b][:, 0:16], Si2[:, tb, 16:32], ALU.mult), [("kn", b), "Si2"], [("kb", b)])
                A("dve", tt(kib[b][:, 0:32], ka[b][:, :], kb[b][:, :], ALU.add), [("ka", b), ("kb", b)], [("kib", b)])
                A("dve", cp(kib[b][:, 32:64], kn[b][:, 32:64]), [("kn", b)], [("kib", b)])
                pt = psbf(6 + b)
                A("pe", tr(pt[0:64, 0:P], kib[b][:, :]), [("kib", b), "ident"], [("ps", 6 + b)])
                A("act", act_fn(kT64[0:64, tb * P:(tb + 1) * P], pt[0:64, 0:P], AF.Copy), [("ps", 6 + b)], ["kT64"])
            S.dma(Q, ki_in, kT64[0:64, :], ["kT64"], ["ki_in"], "c10")
            S.dma(Q, hT_sp, hT[:, :, :].rearrange("p k t -> p (k t)"), HT_ALL, ["hT_sp"], "c11")
            for nm_, a_in, a_out in (("ki", ki_in, ki_all),):
                A("pool", lambda e, a_in=a_in, a_out=a_out: e.collective_compute(
                    "AllGather", ALU.bypass, replica_groups=[list(range(NC))], ins=[a_in], outs=[a_out]),
                  [nm_ + "_in"], [nm_ + "_all"], kind="cc")
            fence(["kt_all", "v_all", "ki_all", "hT_sp"])
            DUMP("KTl", KTl[:, :, :], ["KTl"], BF16)
            DUMP("Vx", Vx[:, :, :, :], ["Vx", "Vx1"], BF16)
            DUMP("kT64", kT64[0:64, :], ["kT64"], BF16)
            DFENCE()
            S.emit()
            if STOP == "A3":
                return nc

    with ExitStack() as es:
        kiT2 = AL(es, "kiT2", [P, 8, TOK], BF16)
        SC = AL(es, "SC", [P, 8192], F32)
        NM = [AL(es, "NM%d" % i, [P, 8192], BF16) for i in range(1)]
        MA = AL(es, "MA", [P, 8, P], F32)
        Kb = [AL(es, "Kb%d" % i, [P, 8, 512], BF16) for i in range(2)]
        Vb = [AL(es, "Vb%d" % i, [P, 4, 1032], BF16) for i in range(2)]
        Z = [AL(es, "Z%d" % i, [P, 512], BF16) for i in range(4)]
        PT = [AL(es, "PT%d" % i, [P, 1024], BF16) for i in range(2)]
        DS = AL(es, "DS", [P, 16, P], BF16)
        yb = AL(es, "yb", [P, 1024], BF16)
        ybT = AL(es, "ybT", [P, 8, P], BF16)
        junkS = AL(es, "junkS", [P, 8192], BF16)
        pw2 = AL(es, "pw2", [P, NIT + 1], F32)
        wtab = AL(es, "wtab", [P, NIT + 1], F32)
        bv = AL(es, "bv", [P, 16], F32)
        rec = AL(es, "rec", [P, 8], F32)
        thrall = AL(es, "thrall", [P, 16], F32)

        kiv = ki_all.rearrange("(r d) t -> d r t", r=8)
        S.dma(Q, kiT2[0:64, :, :], kiv, [], ["kiT2a"], "c0")
        S.dma(Q, kiT2[64:128, :, :], kiv, [], ["kiT2b"], "c1")
        S.dma(Q, pw2[:, :], pow2_d, [], ["pw2"], "c2")
        ktv = kt_all.rearrange("(r h d) t -> d r h t", r=8, h=8)
        vv = v_all.rearrange("(r b s) f -> s r b f", r=8, b=8)
        ybTv = ybT_sp.rearrange("p (j t) -> p j t", j=8)
        kvc = [0]
        zc = [0]
        for lb in range(TB):
            m = lb + 1
            nk = 1024 * m
            for h in range(16):
                A("dve", ts(DS[:, h, :], ident[:, :], sgn[:, lb, h:h + 1], None, ALU.mult), ["ident", ("sgn", lb)], ["DS"])
            S.dma(Q, MA[:, :, :], madd_d[lb], [], ["MA"], "c3")
            chunks = []
            for r in range(8):
                b0 = 0
                while b0 < m:
                    nb = min(4, m - b0)
                    chunks.append((r, b0, nb))
                    b0 += nb
            for ci, (r, b0, nb) in enumerate(chunks):
                N = nb * P
                off = (r * m + b0) * P
                psS = 2
                pend = []
                for h in range(16):
                    pl = h % 2
                    lo_, hi_ = (h % 2) * 64, (h % 2) * 64 + 64
                    A("pe", mm(ps[pl][:, 0:N], qiT[lo_:hi_, lb, h // 2, :], kiT2[lo_:hi_, r, b0 * P:b0 * P + N], True, True),
                      [("qiT", lb), "kiT2a", "kiT2b"], [("ps", pl)])
                    zi = zc[0] % 4
                    zc[0] += 1
                    A("act", act_fn(Z[zi][:, 0:N], ps[pl][:, 0:N], AF.Relu), [("ps", pl)], [("Z", zi)])
                    pend.append((h, zi))
                    if len(pend) > 1:
                        hh, zz = pend.pop(0)
                        A("pe", mm(ps[psS][:, 0:N], DS[:, hh, :], Z[zz][:, 0:N], hh == 0, hh == 15), ["DS", ("Z", zz)], [("ps", psS)])
                for hh, zz in pend:
                    A("pe", mm(ps[psS][:, 0:N], DS[:, hh, :], Z[zz][:, 0:N], hh == 0, hh == 15), ["DS", ("Z", zz)], [("ps", psS)])
                A("dve", cp(SC[:, off:off + N], ps[psS][:, 0:N]), [("ps", psS)], ["SC"])
            hi0 = bv[:, 0:1]
            lo0 = bv[:, 1:2]
            w0 = bv[:, 2:3]
            mid = bv[:, 3:4]
            cntv = bv[:, 4:5]
            dv = bv[:, 5:6]
            thr = bv[:, 6:7]
            A("dve", lambda e, nk=nk: e.tensor_reduce(out=hi0, in_=SC[:, 0:nk], axis=AX.X, op=ALU.max), ["SC"], ["hi0"])
            A("dve", lambda e, nk=nk: e.tensor_reduce(out=lo0, in_=SC[:, 0:nk], axis=AX.X, op=ALU.min), ["SC"], ["lo0"])
            A("dve", ts(lo0, lo0, -1.0, None, ALU.add), ["lo0"], ["lo0"])
            A("dve", tt(w0, hi0, lo0, ALU.subtract), ["hi0", "lo0"], ["w0"])
            A("dve", ts(wtab[:, :], pw2[:, :], w0, None, ALU.mult), ["pw2", "w0"], ["wtab"])
            A("dve", tt(mid, lo0, wtab[:, 0:1], ALU.add), ["lo0", "wtab"], ["mid"])
            scl = SC[:, 0:nk].rearrange("p (r m s) -> p r m s", r=8, m=m)[:, :, m - 1, :]
            A("dve", tt(scl, scl, MA[:, :, :], ALU.add), ["SC", "MA"], ["SC"])
            for it in range(NIT):
                A("dve", lambda e, nk=nk: e.tensor_scalar(out=junkS[:, 0:nk], in0=SC[:, 0:nk], scalar1=mid, scalar2=None,
                                                          op0=ALU.is_ge, op1=ALU.add, accum_out=cntv),
                  ["SC", "mid"], ["junkS", "cnt"])
                A("dve", ts(dv, cntv, 256.0, 0.5, ALU.is_ge, ALU.subtract), ["cnt"], ["dv"])
                A("dve", stt(mid, dv, wtab[:, it:it + 1], mid, ALU.mult, ALU.add), ["dv", "wtab", "mid"], ["mid"])
            A("dve", tt(thr, mid, wtab[:, NIT:NIT + 1], ALU.subtract), ["mid", "wtab"], ["thr"])
            A("dve", ts(NM[0][:, 0:nk], SC[:, 0:nk], thr, None, ALU.is_lt), ["SC", "thr"], ["NM"])
            if lb in (0, 2):
                DUMP("SC%d" % lb, SC[:, 0:nk], ["SC"])
                DUMP("NM%d" % lb, NM[0][:, 0:nk], ["NM"], BF16)
            if debug:
                A("dve", cp(thrall[:, lb:lb + 1], thr), ["thr"], ["thrall"])
                A("dve", cp(thrall[:, 8 + lb:9 + lb], cntv), ["cnt"], ["thrall"])
            ntile = 8 * m
            ti = 0
            for (r, b0, nb) in chunks:
                kb_i = kvc[0] % 2
                kvc[0] += 1
                S.dma(Q, Kb[kb_i][:, :, 0:nb * P], ktv[:, r, :, b0 * P:(b0 + nb) * P], [], [("Kb", kb_i)], ("Kb", kb_i))
                S.dma(Q, Vb[kb_i][:, 0:nb, :], vv[:, r, b0:b0 + nb, :], [], [("Vb", kb_i)], ("Vb", kb_i))
                for j in range(nb):
                    off = (r * m + b0 + j) * P
                    pti = ti % 2
                    for hg in range(2):
                        pa = 3 + hg
                        A("pe", mm(ps[pa][:, :], NM[0][:, off:off + P], negi4[:, :], True, False), ["NM", "negi4"], [("ps", pa)])
                        for hh in range(4):
                            h = hg * 4 + hh
                            A("pe", mm(ps[pa][:, hh * P:(hh + 1) * P], Kb[kb_i][:, h, j * P:(j + 1) * P], qT[:, lb, h, :], False, hh == 3),
                              [("Kb", kb_i), ("qT", lb)], [("ps", pa)])
                        A("act", act_fn(PT[pti][:, hg * 512:(hg + 1) * 512], ps[pa][:, :], AF.Exp, scale=ATT_SCALE),
                          [("ps", pa)], [("PT", pti, hg)])
                    for h in range(8):
                        bank = 5 + h // 3
                        c0 = (h % 3) * 129
                        A("pe", mm(ps[bank][:, c0:c0 + 129], PT[pti][:, h * P:(h + 1) * P], Vb[kb_i][:, j, h * 129:(h + 1) * 129],
                                   ti == 0 and h % 3 == 0, ti == ntile - 1),
                          [("PT", pti, h // 4), ("Vb", kb_i)], [("ps", bank)])
                    ti += 1
            for h in range(8):
                bank = 5 + h // 3
                c0 = (h % 3) * 129
                A("dve", lambda e, h=h, bank=bank, c0=c0: e.reciprocal(out=rec[:, h:h + 1], in_=ps[bank][:, c0 + 128:c0 + 129]),
                  [("ps", bank)], [("rec", h)])
                A("act" if h % 2 else "dve",
                  act_fn(yb[:, h * P:(h + 1) * P], ps[bank][:, c0:c0 + 128], AF.Copy, scale=rec[:, h:h + 1]) if h % 2 else
                  ts(yb[:, h * P:(h + 1) * P], ps[bank][:, c0:c0 + 128], rec[:, h:h + 1], None, ALU.mult),
                  [("ps", bank), ("rec", h)], ["yb"])
            pt = psbf(3)
            for j in range(8):
                A("pe", tr(pt[:, j * P:(j + 1) * P], yb[:, j * P:(j + 1) * P]), ["yb", "ident"], [("ps", 3)])
            A("act", lambda e, pt=pt: e.activation(out=ybT[:, :, :], in_=pt.rearrange("p (j t) -> p j t", j=8), func=AF.Copy),
              [("ps", 3)], ["ybT"])
            S.dma(Q, ybTv[:, :, lb * P:(lb + 1) * P], ybT[:, :, :], ["ybT"], ["ybT_sp"], "c4")
            DUMP("ybT%d" % lb, ybT[:, :, :], ["ybT"], BF16)
        fence(["ybT_sp"])
        DUMP("thr", thrall[:, :], ["thrall"])
        DFENCE()
        S.emit()
        if STOP == "B":
            return nc
    esAB.close()

    with ExitStack() as esC:
        mT = AL(esC, "mT", [P, 16, TOK], BF16)
        with ExitStack() as es:
            hT = AL(es, "hT2", [P, 16, TOK], BF16)
            yaT = AL(es, "yaT2", [P, 8, TOK], BF16)
            ybT = AL(es, "ybT2", [P, 8, TOK], BF16)
            wga = [AL(es, "wga%d" % i, [P, 16, 256], BF16) for i in range(2)]
            wgb = [AL(es, "wgb%d" % i, [P, 16, 256], BF16) for i in range(2)]
            wpa = [AL(es, "wpa%d" % i, [P, 8, 256], BF16) for i in range(2)]
            wpb = [AL(es, "wpb%d" % i, [P, 8, 256], BF16) for i in range(2)]
            sa = [AL(es, "sa%d" % i, [P, 512], BF16) for i in range(2)]
            sbb = [AL(es, "sbb%d" % i, [P, 512], BF16) for i in range(2)]
            m1 = [AL(es, "m1_%d" % i, [P, 512], F32) for i in range(2)]
            m2 = [AL(es, "m2_%d" % i, [P, 512], F32) for i in range(2)]
            S.dma(Q, hT[:, :, :].rearrange("p k t -> p (k t)"), hT_sp, [], ["hT"], "c0")
            S.dma(Q, yaT[:, :, :].rearrange("p k t -> p (k t)"), yaT_sp, [], ["yaT"], "c1")
            S.dma(Q, ybT[:, :, :].rearrange("p k t -> p (k t)"), ybT_sp, [], ["ybT"], "c2")
            wpav = wpa_d.rearrange("(k p) n -> p k n", p=P)
            wpbv = wpb_d.rearrange("(k p) n -> p k n", p=P)
            cnt = 0
            for jg in range(8):
                s = jg % 2
                c0 = jg * 256
                S.dma("pool", wga[s][:, :, :], winv[:, :, COL_GATE + c0:COL_GATE + c0 + 256], [], [("wga", s)], ("wga", s))
                S.dma("pool", wgb[s][:, :, :], winv[:, :, COL_GATE + D + c0:COL_GATE + D + c0 + 256], [], [("wgb", s)], ("wgb", s))
                S.dma("pool", wpa[s][:, :, :], wpav[:, :, c0:c0 + 256], [], [("wpa", s)], ("wpa", s))
                S.dma("pool", wpb[s][:, :, :], wpbv[:, :, c0:c0 + 256], [], [("wpb", s)], ("wpb", s))
                for jj in range(2):
                    j = jg * 2 + jj
                    cs = slice(jj * P, (jj + 1) * P)
                    for hf in range(2):
                        tsl = slice(hf * 512, (hf + 1) * 512)
                        pb0 = (cnt % 2) * 4
                        b = cnt % 2
                        cnt += 1
                        for k in range(16):
                            A("pe", mm(ps[pb0][:, :], wga[s][:, k, cs], hT[:, k, tsl], k == 0, k == 15), [("wga", s), "hT"], [("ps", pb0)])
                        for k in range(16):
                            A("pe", mm(ps[pb0 + 1][:, :], wgb[s][:, k, cs], hT[:, k, tsl], k == 0, k == 15), [("wgb", s), "hT"], [("ps", pb0 + 1)])
                        for k in range(8):
                            A("pe", mm(ps[pb0 + 2][:, :], wpa[s][:, k, cs], yaT[:, k, tsl], k == 0, k == 7), [("wpa", s), "yaT"], [("ps", pb0 + 2)])
                        for k in range(8):
                            A("pe", mm(ps[pb0 + 3][:, :], wpb[s][:, k, cs], ybT[:, k, tsl], k == 0, k == 7), [("wpb", s), "ybT"], [("ps", pb0 + 3)])
                        A("act", act_fn(sa[b][:, :], ps[pb0][:, :], AF.Sigmoid), [("ps", pb0)], [("sa", b)])
                        A("act", act_fn(sbb[b][:, :], ps[pb0 + 1][:, :], AF.Sigmoid), [("ps", pb0 + 1)], [("sbb", b)])
                        A("dve", tt(m1[b][:, :], sa[b][:, :], ps[pb0 + 2][:, :], ALU.mult), [("sa", b), ("ps", pb0 + 2)], [("m1", b)])
                        A("dve", tt(m2[b][:, :], sbb[b][:, :], ps[pb0 + 3][:, :], ALU.mult), [("sbb", b), ("ps", pb0 + 3)], [("m2", b)])
                        A("dve", tt(mT[:, j, tsl], m1[b][:, :], m2[b][:, :], ALU.add), [("m1", b), ("m2", b)], ["mT"])
            DUMP("mT", mT[:, :, :], ["mT"], BF16)
            DFENCE()
            S.emit()
            if STOP == "C1":
                return nc
        h2T = nc.alloc_sbuf_tensor_at("h2T", [P, 16, TOK], BF16, offset=196576)
        with ExitStack() as es:
            wo = AL(es, "wo", [P, 16, D], BF16)
            wov = wo_d.rearrange("(k p) n -> p k n", p=P)
            for g in range(4):
                S.dma("pool", wo[:, :, g * 512:(g + 1) * 512], wov[:, :, g * 512:(g + 1) * 512], [], [("wo", g)], ("wo", g))
            xs = [AL(es, "xs%d" % i, [P, D], F32) for i in range(2)]
            tt_ = [AL(es, "tt%d" % i, [P, D], F32) for i in range(2)]
            xn2 = [AL(es, "xn2%d" % i, [P, D], BF16) for i in range(2)]
            gmb = AL(es, "gmb2", [P, D], F32)
            junk = AL(es, "junkc", [P, D], BF16)
            S.dma(Q, gmb[:, :], gmb_sp, [], ["gmb"], "c0")
            for tb in range(TB):
                b = tb % 2
                S.dma(Q, xs[b][:, :], x_d[tb * P:(tb + 1) * P, :], [], [("xs", b)], ("xs", b))
                pb0 = b * 4
                for cg in range(4):
                    for k in range(16):
                        A("pe", mm(ps[pb0 + cg][:, :], mT[:, k, tb * P:(tb + 1) * P], wo[:, k, cg * 512:(cg + 1) * 512], k == 0, k == 15),
                          [("wo", cg)], [("ps", pb0 + cg)])
                sq4 = st[:, 12 + 4 * b:16 + 4 * b]
                A("dve", lambda e, o=sq4: e.memset(o, 0.0), [], [("sq4", b)])
                for cg in range(4):
                    A("act", act_fn(junk[:, cg * 512:(cg + 1) * 512], ps[pb0 + cg][:, :], AF.Square, accum_out=sq4[:, cg:cg + 1]),
                      [("ps", pb0 + cg), ("sq4", b)], ["junk", ("sq4", b)])
                ssq = st[:, b:b + 1]
                A("dve", lambda e, o=ssq, i=sq4: e.tensor_reduce(out=o, in_=i, axis=AX.X, op=ALU.add), [("sq4", b)], [("ssq", b)])
                r, rk = rstd_chain(ssq, ("ssq", b), D, b)
                for cg in range(4):
                    sl = slice(cg * 512, (cg + 1) * 512)
                    A("dve", ts(tt_[b][:, sl], ps[pb0 + cg][:, :], r, None, ALU.mult), [("ps", pb0 + cg), rk], [("tt", b, cg)])
                    A("dve", tt(tt_[b][:, sl], tt_[b][:, sl], gmb[:, sl], ALU.mult), [("tt", b, cg), "gmb"], [("tt", b, cg)])
                    A("dve", tt(xs[b][:, sl], xs[b][:, sl], tt_[b][:, sl], ALU.add), [("tt", b, cg), ("xs", b)], [("xs", b)])
                S.dma(Q, x1_sp[tb * P:(tb + 1) * P, :], xs[b][:, :], [("xs", b)], ["x1_sp"], ("x1s", b))
                ssq2 = st[:, 2 + b:3 + b]
                A("dve", lambda e, o=ssq2: e.memset(o, 0.0), [], [("ssq2", b)])
                A("act", act_fn(junk[:, :], xs[b][:, :], AF.Square, accum_out=ssq2), [("xs", b), ("ssq2", b)], ["junk", ("ssq2", b)])
                r2, rk2 = rstd_chain(ssq2, ("ssq2", b), D, b)
                A("dve", ts(xn2[b][:, :], xs[b][:, :], r2, None, ALU.mult), [("xs", b), rk2], [("xn2", b)])
                for g4 in range(4):
                    pbk = (1 - b) * 4 + g4
                    pt = psbf(pbk)
                    for i in range(4):
                        k = g4 * 4 + i
                        A("pe", tr(pt[:, i * P:(i + 1) * P], xn2[b][:, k * P:(k + 1) * P]), [("xn2", b), "ident"], [("ps", pbk)])
                    for i in range(4):
                        k = g4 * 4 + i
                        o = h2T[:, k, tb * P:(tb + 1) * P]
                        if i % 2 == 0:
                            A("dve", ts(o, pt[:, i * P:(i + 1) * P], gsf_c[:, k:k + 1], shf_c[:, k:k + 1], ALU.mult, ALU.add),
                              [("ps", pbk)], ["h2T"])
                        else:
                            A("act", act_fn(o, pt[:, i * P:(i + 1) * P], AF.Identity, scale=gsf_c[:, k:k + 1], bias=shf_c[:, k:k + 1]),
                              [("ps", pbk)], ["h2T"])
            fence(["x1_sp"])
            DUMP("h2T", h2T[:, :, :], ["h2T"], BF16)
            DFENCE()
            S.emit()
            if STOP == "C2":
                return nc

    with ExitStack() as esF:
        aT = AL(esF, "aT", [P, 44, TOK], BF16)
        with ExitStack() as es:
            wG = [AL(es, "wG%d" % i, [P, 16, 256], BF16) for i in range(2)]
            wU = [AL(es, "wU%d" % i, [P, 16, 256], BF16) for i in range(2)]
            sgl = [AL(es, "sgl%d" % i, [P, 512], F32) for i in range(2)]
            wgv = wg_d.rearrange("(k p) n -> p k n", p=P)
            wuv = wu_d.rearrange("(k p) n -> p k n", p=P)
            cnt = 0
            for jg in range(22):
                s = jg % 2
                S.dma("pool", wG[s][:, :, :], wgv[:, :, jg * 256:(jg + 1) * 256], [], [("wG", s)], ("wG", s))
                S.dma("pool", wU[s][:, :, :], wuv[:, :, jg * 256:(jg + 1) * 256], [], [("wU", s)], ("wU", s))
                for jj in range(2):
                    j = jg * 2 + jj
                    cs = slice(jj * P, (jj + 1) * P)
                    for hf in range(2):
                        tsl = slice(hf * 512, (hf + 1) * 512)
                        pg = (cnt % 4) * 2
                        b = cnt % 2
                        cnt += 1
                        for k in range(16):
                            A("pe", mm(ps[pg][:, :], wG[s][:, k, cs], h2T[:, k, tsl], k == 0, k == 15), [("wG", s)], [("ps", pg)])
                        for k in range(16):
                            A("pe", mm(ps[pg + 1][:, :], wU[s][:, k, cs], h2T[:, k, tsl], k == 0, k == 15), [("wU", s)], [("ps", pg + 1)])
                        A("act", act_fn(sgl[b][:, :], ps[pg][:, :], AF.Silu), [("ps", pg)], [("sgl", b)])
                        A("dve", tt(aT[:, j, tsl], sgl[b][:, :], ps[pg + 1][:, :], ALU.mult), [("sgl", b), ("ps", pg + 1)], ["aT"])
            S.emit()
            if STOP == "D":
                return nc
        with ExitStack() as es:
            wd = [AL(es, "wd%d" % i, [P, 4, 512], BF16) for i in range(3)]
            f = AL(es, "f", [P, TB, D], F32)
            xs = [AL(es, "xs%d" % i, [P, D], F32) for i in range(2)]
            gfb = AL(es, "gfb2", [P, D], F32)
            junk = AL(es, "junke", [P, D], BF16)
            S.dma(Q, gfb[:, :], gfb_sp, [], ["gfb"], "c0")
            wdv = wd_d.rearrange("(k p) n -> p k n", p=P)
            wc = 0
            for cg in range(4):
                for kg in range(11):
                    s = wc % 3
                    wc += 1
                    S.dma("pool", wd[s][:, :, :], wdv[:, kg * 4:(kg + 1) * 4, cg * 512:(cg + 1) * 512], [], [("wd", s)], ("wd", s))
                    for kk in range(4):
                        k = kg * 4 + kk
                        for tb in range(TB):
                            A("pe", mm(ps[tb][:, :], aT[:, k, tb * P:(tb + 1) * P], wd[s][:, kk, :], k == 0, k == 43), [("wd", s)], [("ps", tb)])
                for tb in range(TB):
                    if tb % 2:
                        A("act", act_fn(f[:, tb, cg * 512:(cg + 1) * 512], ps[tb][:, :], AF.Copy), [("ps", tb)], [("f", tb)])
                    else:
                        A("dve", cp(f[:, tb, cg * 512:(cg + 1) * 512], ps[tb][:, :]), [("ps", tb)], [("f", tb)])
            for tb in range(TB):
                b = tb % 2
                S.dma(Q, xs[b][:, :], x1_sp[tb * P:(tb + 1) * P, :], [], [("xs", b)], ("xs", b))
                ssq = st[:, b:b + 1]
                A("dve", lambda e, o=ssq: e.memset(o, 0.0), [], [("ssq", b)])
                A("act", act_fn(junk[:, :], f[:, tb, :], AF.Square, accum_out=ssq), [("f", tb), ("ssq", b)], ["junk", ("ssq", b)])
                r, rk = rstd_chain(ssq, ("ssq", b), D, b)
                A("dve", ts(f[:, tb, :], f[:, tb, :], r, None, ALU.mult), [("f", tb), rk], [("f", tb)])
                A("dve", tt(f[:, tb, :], f[:, tb, :], gfb[:, :], ALU.mult), [("f", tb), "gfb"], [("f", tb)])
                A("dve", tt(xs[b][:, :], xs[b][:, :], f[:, tb, :], ALU.add), [("f", tb), ("xs", b)], [("xs", b)])
                S.dma(Q, out_d[tb * P:(tb + 1) * P, :], xs[b][:, :], [("xs", b)], ["out"], ("os", b))
            fence(["out"])
            S.emit()
            if STOP == "E":
                return nc
    return nc


_CACHE = {}


def _rope_tabs(pos, dim):
    inv = (1.0 / (10000.0 ** (np.arange(0, dim, 2, dtype=np.float32) / np.float32(dim)))).astype(np.float32)
    ang = pos.astype(np.float32)[:, None] * inv[None, :]
    return np.cos(ang).astype(np.float32), np.sin(ang).astype(np.float32)


def kernel(**inp):
    f32 = np.float32
    if "nc" not in _CACHE:
        _CACHE["nc"] = build_program(DEBUG)
    nc = _CACHE["nc"]
    x = np.asarray(inp["x"], f32)[0]
    g = lambda k: np.ascontiguousarray(np.asarray(inp[k], f32)[0])
    col = lambda v, n: np.ascontiguousarray(v.reshape(n, P).T)
    shared = {
        "c_col": col(np.asarray(inp["c"], f32)[0], 16),
        "g_pre_mix_col": col(g("g_pre_mix"), 16),
        "g_pre_ffn_col": col(g("g_pre_ffn"), 16),
        "g_post_mix": g("g_post_mix")[None, :],
        "g_post_ffn": g("g_post_ffn")[None, :],
        "w_in": g("w_in"),
        "gmlp_ln_g": g("gmlp_ln_g")[None, :],
        "gmlp_ln_b": g("gmlp_ln_b")[None, :],
        "gmlp_wsT": np.ascontiguousarray(g("gmlp_w_s").transpose(2, 0, 1)),
        "gmlp_bs_col": np.ascontiguousarray(g("gmlp_b_s").T),
        "q_lat_g_col": col(g("q_lat_norm_g"), 4),
        "w_q_up": g("w_q_up"), "w_qidx_up": g("w_qidx_up"),
        "kidx_ln_g": g("kidx_ln_g")[None, :], "kidx_ln_b": g("kidx_ln_b")[None, :],
        "w_proj_a": g("w_proj_a"), "w_proj_b": g("w_proj_b"), "w_out": g("w_out"),
        "w_ffn_gate": g("w_ffn_gate"), "w_ffn_up": g("w_ffn_up"), "w_ffn_down": g("w_ffn_down"),
        "ident_bf": np.eye(P, dtype=f32).astype(ml_dtypes.bfloat16),
        "negi4": np.tile(np.eye(P, dtype=f32) * -30000.0, (1, 4)).astype(ml_dtypes.bfloat16),
        "causalT": np.triu(np.ones((P, P), f32)),
        "pow2": np.tile((0.5 ** np.arange(1, NIT + 2, dtype=np.float64)).astype(f32)[None, :], (P, 1)),
    }
    w_mod = g("w_mod")
    b_mod = g("b_mod")
    in_maps = []
    tri = np.where(np.arange(P)[None, :] <= np.arange(P)[:, None], 0.0, NEG).astype(f32)
    for c in range(NC):
        blks = core_blocks(c)
        rows = np.concatenate([np.arange(b * P, (b + 1) * P) for b in blks])
        pos = rows.reshape(TB, P)
        ca, sa = _rope_tabs(rows, 128)
        ci, si = _rope_tabs(rows, 32)
        c2 = np.concatenate([ca, ca], 1).reshape(TB, P, 128).transpose(1, 0, 2)
        s2 = np.concatenate([-sa, sa], 1).reshape(TB, P, 128).transpose(1, 0, 2)
        ci2 = np.concatenate([ci, ci], 1).reshape(TB, P, 32).transpose(1, 0, 2)
        si2 = np.concatenate([-si, si], 1).reshape(TB, P, 32).transpose(1, 0, 2)
        madd = np.zeros((TB, P, 8, P), f32)
        for lb in range(TB):
            qb = blks[lb]
            for r in range(NC):
                kb_ = core_blocks(r)[lb]
                if kb_ > qb:
                    madd[lb, :, r, :] = NEG
                elif kb_ == qb:
                    madd[lb, :, r, :] = tri
        m = dict(shared)
        m.update({
            "x": np.ascontiguousarray(x[rows]),
            "w_mod_sl": np.ascontiguousarray(w_mod[:, c * 1536:(c + 1) * 1536]),
            "b_mod_sl": np.ascontiguousarray(b_mod[None, c * 1536:(c + 1) * 1536]),
            "rope_c2": np.ascontiguousarray(c2), "rope_s2": np.ascontiguousarray(s2),
            "rope_ci2": np.ascontiguousarray(ci2), "rope_si2": np.ascontiguousarray(si2),
            "maskadd": madd,
        })
        in_maps.append(m)
    res = run_bass_kernel_spmd(nc, in_maps, core_ids=list(range(NC)))
    if DEBUG:
        _DBG["res"] = res.results
    out = np.empty((8192, D), f32)
    for c in range(NC):
        blks = core_blocks(c)
        o = np.asarray(res.results[c]["out"], f32)
        for lb, b in enumerate(blks):
            out[b * P:(b + 1) * P] = o[lb * P:(lb + 1) * P]
    return out[None]
```

```python
import numpy as np
import ml_dtypes
import concourse.bass as bass
import concourse.mybir as mybir
from concourse.bass_utils import run_bass_kernel_spmd

F32 = mybir.dt.float32
BF16 = mybir.dt.bfloat16
U32 = mybir.dt.uint32
AF = mybir.ActivationFunctionType
ALU = mybir.AluOpType
AX = mybir.AxisListType

NC = 8
P = 128
TB = 8
TOK = 1024
D = 2048
HID = 5632
NIT = 20
COL_QLAT, COL_K, COL_V, COL_KIDX, COL_IDXW, COL_GATE = 2048, 2560, 3584, 4608, 4672, 4688
IN_COLS = 8784
EPS = 1e-6
NEG = -1.0e30
IDXW_SCALE = (16 ** -0.5) * (64 ** -0.5)
ATT_SCALE = 128 ** -0.5


def core_blocks(c):
    return sorted([16 * j + c for j in range(4)] + [16 * j + 15 - c for j in range(4)])


class _Op:
    __slots__ = ("eng", "fn", "deps", "kind", "semkey", "token", "need", "eidx", "inc")


class Sched:
    def __init__(self, nc):
        self.nc = nc
        self.engs = {"pe": nc.tensor, "act": nc.scalar, "dve": nc.vector, "pool": nc.gpsimd, "sp": nc.sync}
        self.esem = {e: nc.alloc_semaphore("es_" + e) for e in self.engs}
        self.ecount = {e: 0 for e in self.engs}
        self.dsem = {}
        self.dcount = {}
        self.dlast = {}
        self.ncc = 0
        self._reset()

    def _reset(self):
        self.ops = []
        self.lastw = {}
        self.readers = {}
        self.eops = {e: 0 for e in self.engs}

    def add(self, eng, fn, reads=(), writes=(), kind="c", semkey=None):
        op = _Op()
        op.eng, op.fn, op.kind, op.semkey = eng, fn, kind, semkey
        op.need = False
        op.token = None
        deps = []
        for r in reads:
            w = self.lastw.get(r)
            if w is not None:
                deps.append(w)
        for r in writes:
            w = self.lastw.get(r)
            if w is not None:
                deps.append(w)
            for rd in self.readers.get(r, {}).values():
                deps.append(rd)
        if kind == "dma":
            prev = self.dlast.get(semkey)
            if prev is not None:
                deps.append(prev)
            self.dlast[semkey] = op
        op.deps = deps
        rk = eng if kind == "c" else (kind, semkey, id(op))
        for r in reads:
            self.readers.setdefault(r, {})[rk] = op
        for r in writes:
            self.lastw[r] = op
            self.readers[r] = {}
        op.eidx = self.eops[eng]
        self.eops[eng] += 1
        self.ops.append(op)
        return op

    def dma(self, q, out, in_, reads, writes, semkey):
        return self.add(q, lambda e, o=out, i=in_: e.dma_start(out=o, in_=i), reads, writes, "dma", semkey)

    def emit(self):
        nc = self.nc
        ops = self.ops
        phase_ops = set(id(o) for o in ops)
        for op in ops:
            nd = []
            seen = set()
            for d in op.deps:
                if id(d) in seen or d is op:
                    continue
                seen.add(id(d))
                if id(d) not in phase_ops:
                    continue
                if d.kind == "c" and d.eng == op.eng:
                    if op.eng == "pe":
                        continue
                    if op.eidx - d.eidx > 2:
                        continue
                nd.append(d)
                d.need = True
            op.deps = nd
        for op in ops:
            if op.kind == "dma":
                if op.semkey not in self.dsem:
                    self.dsem[op.semkey] = nc.alloc_semaphore("ds%d" % len(self.dsem))
                    self.dcount[op.semkey] = 0
                self.dcount[op.semkey] += 16
                op.token = (self.dsem[op.semkey], self.dcount[op.semkey])
                op.inc = 16
            elif op.kind == "cc":
                s = nc.alloc_semaphore("cc%d" % self.ncc)
                self.ncc += 1
                op.token = (s, 1)
                op.inc = 1
            elif op.need:
                self.ecount[op.eng] += 1
                op.token = (self.esem[op.eng], self.ecount[op.eng])
                op.inc = 1
        per = {e: [o for o in ops if o.eng == e] for e in self.engs}
        waited = {e: {} for e in self.engs}

        def run(e, lst, ename):
            wd = waited[ename]
            for op in lst:
                for d in op.deps:
                    sem, val = d.token
                    if wd.get(sem.num, 0) >= val:
                        continue
                    wd[sem.num] = val
                    e.wait_ge(sem, val)
                ins = op.fn(e)
                if op.token is not None:
                    ins.then_inc(op.token[0], op.inc)

        with nc.Block() as block:
            if per["pe"]:
                @block.tensor
                def _(e):
                    run(e, per["pe"], "pe")
            if per["act"]:
                @block.scalar
                def _(e):
                    run(e, per["act"], "act")
            if per["dve"]:
                @block.vector
                def _(e):
                    run(e, per["dve"], "dve")
            if per["pool"]:
                @block.gpsimd
                def _(e):
                    run(e, per["pool"], "pool")
            if per["sp"]:
                @block.sync
                def _(e):
                    run(e, per["sp"], "sp")
        self._reset()


DEBUG = False
DBG_NAMES = None
STOP = None
_DBG = {}


def build_program(debug=False):
    nc = bass.Bass("TRN2", target_bir_lowering=False)
    S = Sched(nc)
    dbg_keys = []

    def DUMP(name, src, reads, dt=F32):
        if not debug or (DBG_NAMES is not None and name not in DBG_NAMES):
            return
        t = nc.dram_tensor("dbg_" + name, list(src.shape), dt, kind="ExternalOutput").ap()
        S.dma("sp", t, src, list(reads), ["dbg_" + name], ("dbg", name))
        dbg_keys.append("dbg_" + name)

    def DFENCE():
        if debug and dbg_keys:
            S.add("dve", lambda e: e.memset(st[:, 50:51], 0.0), list(dbg_keys), ["st50"])
            del dbg_keys[:]

    def din(name, shape, dt=F32):
        return nc.dram_tensor(name, list(shape), dt, kind="ExternalInput").ap()

    x_d = din("x", [TOK, D])
    ccol_d = din("c_col", [P, 16])
    wmod_d = din("w_mod_sl", [D, 1536])
    bmod_d = din("b_mod_sl", [1, 1536])
    gpm_d = din("g_pre_mix_col", [P, 16])
    gpf_d = din("g_pre_ffn_col", [P, 16])
    gqm_d = din("g_post_mix", [1, D])
    gqf_d = din("g_post_ffn", [1, D])
    win_d = din("w_in", [D, IN_COLS])
    lng_d = din("gmlp_ln_g", [1, 1024])
    lnb_d = din("gmlp_ln_b", [1, 1024])
    wsT_d = din("gmlp_wsT", [P, 8, P])
    bsc_d = din("gmlp_bs_col", [P, 8])
    qg_d = din("q_lat_g_col", [P, 4])
    wq_d = din("w_q_up", [512, 1024])
    wqi_d = din("w_qidx_up", [512, 1024])
    kig_d = din("kidx_ln_g", [1, 64])
    kib_d = din("kidx_ln_b", [1, 64])
    wpa_d = din("w_proj_a", [1024, D])
    wpb_d = din("w_proj_b", [1024, D])
    wo_d = din("w_out", [D, D])
    wg_d = din("w_ffn_gate", [D, HID])
    wu_d = din("w_ffn_up", [D, HID])
    wd_d = din("w_ffn_down", [HID, D])
    c2_d = din("rope_c2", [P, TB, 128])
    s2_d = din("rope_s2", [P, TB, 128])
    ci2_d = din("rope_ci2", [P, TB, 32])
    si2_d = din("rope_si2", [P, TB, 32])
    madd_d = din("maskadd", [TB, P, 8, P])
    ident_d = din("ident_bf", [P, P], BF16)
    negi_d = din("negi4", [P, 512], BF16)
    causT_d = din("causalT", [P, P])
    pow2_d = din("pow2", [P, NIT + 1])
    out_d = nc.dram_tensor("out", [TOK, D], F32, kind="ExternalOutput").ap()

    def dint(name, shape, dt):
        return nc.dram_tensor(name, list(shape), dt).ap()

    mod_in = dint("mod_in", [1, 1536], F32)
    mod_all = dint("mod_all", [8, 1536], F32)
    kt_in = dint("kt_in", [1024, 1024], BF16)
    kt_all = dint("kt_all", [8 * 1024, 1024], BF16)
    v_in = dint("v_in", [1024, 1032], BF16)
    v_all = dint("v_all", [8 * 1024, 1032], BF16)
    ki_in = dint("ki_in", [64, 1024], BF16)
    ki_all = dint("ki_all", [8 * 64, 1024], BF16)
    hT_sp = dint("hT_sp", [P, 16 * 1024], BF16)
    yaT_sp = dint("yaT_sp", [P, 8 * 1024], BF16)
    ybT_sp = dint("ybT_sp", [P, 8 * 1024], BF16)
    x1_sp = dint("x1_sp", [TOK, D], F32)
    gmb_sp = dint("gmb_sp", [P, D], F32)
    gfb_sp = dint("gfb_sp", [P, D], F32)

    def sb(name, shape, dt):
        return nc.alloc_sbuf_tensor(name, list(shape), dt)

    ident = sb("ident", [P, P], BF16)
    negi4 = sb("negi4s", [P, 512], BF16)
    onesb = sb("onesb", [P, 1], BF16)
    gsm_c = sb("gsm_c", [P, 16], F32)
    shm_c = sb("shm_c", [P, 16], F32)
    gsf_c = sb("gsf_c", [P, 16], F32)
    shf_c = sb("shf_c", [P, 16], F32)
    sgn = sb("sgn", [P, TB, 16], F32)
    wabs = sb("wabs", [P, TB, 16], F32)
    KI = sb("KI", [P, TB, 80], F32)
    st = sb("stats", [P, 64], F32)

    ps = [nc.alloc_psum_tensor("ps%d" % i, [P, 512], F32) for i in range(8)]

    def psbf(i):
        return ps[i][:, :].bitcast(BF16)

    A = S.add
    Q = "sp"

    def act_fn(out, in_, func, **kw):
        return lambda e: e.activation(out=out, in_=in_, func=func, **kw)

    def ts(out, in0, s1, s2, op0, op1=None, **kw):
        if op1 is None:
            return lambda e: e.tensor_scalar(out=out, in0=in0, scalar1=s1, scalar2=None, op0=op0, **kw)
        return lambda e: e.tensor_scalar(out=out, in0=in0, scalar1=s1, scalar2=s2, op0=op0, op1=op1, **kw)

    def tt(out, in0, in1, op):
        return lambda e: e.tensor_tensor(out=out, in0=in0, in1=in1, op=op)

    def stt(out, in0, scalar, in1, op0, op1):
        return lambda e: e.scalar_tensor_tensor(out=out, in0=in0, scalar=scalar, in1=in1, op0=op0, op1=op1)

    def mm(out, lhsT, rhs, start, stop):
        return lambda e: e.matmul(out, lhsT, rhs, start=start, stop=stop)

    def tr(out, in_):
        return lambda e: e.transpose(out, in_, ident[:, :])

    def cp(out, in_):
        return lambda e: e.tensor_copy(out=out, in_=in_)

    def rstd_chain(ssq_ap, n, key):
        t1 = st[:, 60:61]
        t2 = st[:, 61:62]
        r = st[:, 62:63]
        A("dve", ts(t1, ssq_ap, 1.0 / n, EPS, ALU.mult, ALU.add), [key + "_ssq"], ["st60"])
        A("act", act_fn(t2, t1, AF.Sqrt), ["st60"], ["st61"])
        A("dve", lambda e: e.reciprocal(out=r, in_=t2), ["st61"], [key])
        return r

    from contextlib import ExitStack

    def rstd_chain(ssq_ap, ssq_key, n, par=0):
        c0 = 52 + 4 * par
        t1 = st[:, c0:c0 + 1]
        t2 = st[:, c0 + 1:c0 + 2]
        r = st[:, c0 + 2:c0 + 3]
        k = "rs%d_" % par
        A("dve", ts(t1, ssq_ap, 1.0 / n, EPS, ALU.mult, ALU.add), [ssq_key], [k + "a"])
        A("act", act_fn(t2, t1, AF.Sqrt), [k + "a"], [k + "b"])
        A("dve", lambda e: e.reciprocal(out=r, in_=t2), [k + "b"], [k + "r"])
        return r, k + "r"

    def fence(keys):
        A("dve", lambda e: e.memset(st[:, 63:64], 0.0), list(keys), ["st63"])

    _aln = [0]

    def AL(es, name, shape, dt):
        _aln[0] += 1
        return es.enter_context(nc.sbuf_tensor("%s_%d" % (name, _aln[0]), list(shape), dt))

    def bcast_row2(ap2d):
        return ap2d.partition_broadcast(P).rearrange("p o n -> p (o n)")

    with ExitStack() as es:
        wm = AL(es, "wm", [P, 16, 1536], F32)
        ccol = AL(es, "ccol", [P, 16], F32)
        bm = AL(es, "bm", [1, 1536], F32)
        modrow = AL(es, "modrow", [1, 1536], F32)
        gmb = AL(es, "gmb", [P, D], F32)
        gfb = AL(es, "gfb", [P, D], F32)
        gq = AL(es, "gq", [P, D], F32)
        mcols = AL(es, "mcols", [P, 4, 16], F32)
        gpc = AL(es, "gpc", [P, 2, 16], F32)

        S.dma(Q, ident[:, :], ident_d, [], ["ident"], "c0")
        S.dma(Q, negi4[:, :], negi_d, [], ["negi4"], "c1")
        A("dve", lambda e: e.memset(onesb[:, :], 1.0), [], ["onesb"])
        S.dma(Q, ccol[:, :], ccol_d, [], ["ccol"], "c2")
        S.dma(Q, bm[:, :], bmod_d, [], ["bm"], "c3")
        wmv = wmod_d.rearrange("(k p) n -> p k n", p=P)
        for g in range(4):
            S.dma(Q, wm[:, 4 * g:4 * g + 4, :], wmv[:, 4 * g:4 * g + 4, :], [], [("wm", g)], ("wm", g))
        for n in range(3):
            for k in range(16):
                A("pe", mm(ps[n][0:1, :], ccol[:, k:k + 1], wm[:, k, n * 512:(n + 1) * 512], k == 0, k == 15),
                  ["ccol", ("wm", k // 4)], [("ps", n)])
            A("dve", tt(modrow[0:1, n * 512:(n + 1) * 512], ps[n][0:1, :], bm[0:1, n * 512:(n + 1) * 512], ALU.add),
              [("ps", n), "bm"], ["modrow"])
        S.dma(Q, mod_in, modrow[0:1, :], ["modrow"], ["mod_in"], "c4")
        A("pool", lambda e: e.collective_compute("AllGather", ALU.bypass, replica_groups=[list(range(NC))],
                                                 ins=[mod_in], outs=[mod_all]),
          ["mod_in"], ["mod_all"], kind="cc")
        modflat = mod_all.rearrange("a n -> (a n)")

        def mslice(i):
            return modflat[i * D:(i + 1) * D]

        def bcast_row(ap1d):
            return bcast_row2(ap1d.rearrange("(o n) -> o n", o=1))

        S.dma(Q, gmb[:, :], bcast_row(mslice(2)), ["mod_all"], ["gmb"], "c5")
        S.dma(Q, gfb[:, :], bcast_row(mslice(5)), ["mod_all"], ["gfb"], "c6")
        for i, mi in enumerate([0, 1, 3, 4]):
            src = mslice(mi).rearrange("(k p) -> p k", p=P)
            A(Q, lambda e, o=mcols[:, i, :], s=src: e.dma_start(out=o, in_=s, allow_slow_non_contiguous=True),
              ["mod_all"], [("mcols", i)], "dma", ("mc", i))
        S.dma(Q, gpc[:, 0, :], gpm_d, [], ["gpc0"], "c7")
        S.dma(Q, gpc[:, 1, :], gpf_d, [], ["gpc1"], "c8")
        A("dve", stt(gsm_c[:, :], mcols[:, 1, :], 1.0, gpc[:, 0, :], ALU.add, ALU.mult), [("mcols", 1), "gpc0"], ["gsm"])
        A("dve", stt(gsf_c[:, :], mcols[:, 3, :], 1.0, gpc[:, 1, :], ALU.add, ALU.mult), [("mcols", 3), "gpc1"], ["gsf"])
        A("dve", cp(shm_c[:, :], mcols[:, 0, :]), [("mcols", 0)], ["shm"])
        A("dve", cp(shf_c[:, :], mcols[:, 2, :]), [("mcols", 2)], ["shf"])
        S.dma(Q, gq[:, :], bcast_row2(gqm_d), [], ["gq"], "c9")
        A("dve", tt(gmb[:, :], gmb[:, :], gq[:, :], ALU.mult), ["gmb", "gq"], ["gmb"])
        S.dma(Q, gmb_sp, gmb[:, :], ["gmb"], ["gmb_sp"], "c5")
        S.dma(Q, gq[:, :], bcast_row2(gqf_d), ["gq"], ["gq"], "c9")
        A("dve", tt(gfb[:, :], gfb[:, :], gq[:, :], ALU.mult), ["gfb", "gq"], ["gfb"])
        S.dma(Q, gfb_sp, gfb[:, :], ["gfb"], ["gfb_sp"], "c6")
        fence(["gmb_sp", "gfb_sp"])
        DUMP("gsm", gsm_c[:, :], ["gsm"]); DUMP("shm", shm_c[:, :], ["shm"]); DUMP("gsf", gsf_c[:, :], ["gsf"]); DUMP("shf", shf_c[:, :], ["shf"])
        DUMP("gmb", gmb[:, :], ["gmb"]); DUMP("gfb", gfb[:, :], ["gfb"])
        DFENCE()
        S.emit()
        if STOP == "M":
            return nc

    winv = win_d.rearrange("(k p) n -> p k n", p=P)
    esAB = ExitStack()
    qT = AL(esAB, "qT", [P, TB, 8, P], BF16)
    qiT = AL(esAB, "qiT", [P, TB, 8, P], BF16)
    with ExitStack() as esA:
        hT = AL(esA, "hT", [P, 16, TOK], BF16)
        with ExitStack() as es:
            xs = [AL(es, "xs%d" % i, [P, D], F32) for i in range(2)]
            xn = [AL(es, "xn%d" % i, [P, D], BF16) for i in range(2)]
            junk = AL(es, "junk", [P, D], BF16)
            for tb in range(TB):
                b = tb % 2
                S.dma(Q, xs[b][:, :], x_d[tb * P:(tb + 1) * P, :], [], [("xs", b)], ("xs", b))
                ssq = st[:, b:b + 1]
                A("dve", lambda e, o=ssq: e.memset(o, 0.0), [], [("ssq", b)])
                A("act", act_fn(junk[:, :], xs[b][:, :], AF.Square, accum_out=ssq), [("xs", b), ("ssq", b)], ["junk", ("ssq", b)])
                r, rk = rstd_chain(ssq, ("ssq", b), D, b)
                A("dve", ts(xn[b][:, :], xs[b][:, :], r, None, ALU.mult), [("xs", b), rk], [("xn", b)])
                for g4 in range(4):
                    pb = 4 + (tb * 4 + g4) % 4
                    pt = psbf(pb)
                    for i in range(4):
                        k = g4 * 4 + i
                        A("pe", tr(pt[:, i * P:(i + 1) * P], xn[b][:, k * P:(k + 1) * P]), [("xn", b), "ident"], [("ps", pb)])
                    for i in range(4):
                        k = g4 * 4 + i
                        o = hT[:, k, tb * P:(tb + 1) * P]
                        if i % 2 == 0:
                            A("dve", ts(o, pt[:, i * P:(i + 1) * P], gsm_c[:, k:k + 1], shm_c[:, k:k + 1], ALU.mult, ALU.add),
                              [("ps", pb), "gsm", "shm"], [("hT", tb)])
                        else:
                            A("act", act_fn(o, pt[:, i * P:(i + 1) * P], AF.Identity, scale=gsm_c[:, k:k + 1], bias=shm_c[:, k:k + 1]),
                              [("ps", pb), "gsm", "shm"], [("hT", tb)])
            DUMP("hT", hT[:, :, :], [("hT", t_) for t_ in range(TB)], BF16)
            DFENCE()
            S.emit()
            if STOP == "A0":
                return nc

        HT_ALL = [("hT", tb) for tb in range(TB)]
        wcount = [0]

        def load_w(ring, c0, ncol, key):
            i = wcount[0] % len(ring)
            wcount[0] += 1
            S.dma("pool", ring[i][:, :, 0:ncol], winv[:, :, c0:c0 + ncol], [], [(key, i)], (key, i))
            return ring[i], (key, i)

        def proj_tok(wt, wkey, ncol, tb, pbank, col0=0):
            for k in range(16):
                A("pe", mm(ps[pbank][:, 0:ncol], hT[:, k, tb * P:(tb + 1) * P], wt[:, k, col0:col0 + ncol], k == 0, k == 15),
                  [("hT", tb), wkey], [("ps", pbank)])

        with ExitStack() as es:
            ring = [AL(es, "wr%d" % i, [P, 16, 512], BF16) for i in range(2)]
            U = AL(es, "U", [P, TB, 1024], BF16)
            Vr = AL(es, "Vr", [P, TB, 1024], F32)
            x2 = [AL(es, "x2_%d" % i, [P, 512], F32) for i in range(1)]
            inn = [AL(es, "inn%d" % i, [P, 512], F32) for i in range(1)]
            sg = [AL(es, "sg%d" % i, [P, 512], F32) for i in range(1)]
            vhat = [AL(es, "vhat%d" % i, [P, 1024], BF16) for i in range(1)]
            t1 = [AL(es, "t1_%d" % i, [P, 1024], F32) for i in range(1)]
            ya = [AL(es, "ya%d" % i, [P, 1024], BF16) for i in range(1)]
            junk1 = AL(es, "junk1", [P, 1024], BF16)
            wsf = AL(es, "wsf", [P, 8, P], F32)
            WsT = AL(es, "WsT", [P, 8, P], BF16)
            causT = AL(es, "causT", [P, P], F32)
            LG = AL(es, "LG", [P, 1024], F32)
            LB = AL(es, "LB", [P, 1024], F32)
            B0 = AL(es, "B0", [P, 1024], F32)
            bsc = AL(es, "bsc", [P, 8], F32)
            rsum = AL(es, "rsum", [P, 8], F32)
            yaT = AL(es, "yaT", [P, 8, TOK], BF16)

            S.dma(Q, wsf[:, :, :], wsT_d, [], ["wsf"], "c0")
            S.dma(Q, causT[:, :], causT_d, [], ["causT"], "c1")
            S.dma(Q, LG[:, :], bcast_row2(lng_d), [], ["LG"], "c2")
            S.dma(Q, LB[:, :], bcast_row2(lnb_d), [], ["LB"], "c3")
            S.dma(Q, bsc[:, :], bsc_d, [], ["bsc"], "c4")
            A("dve", tt(WsT[:, :, :], wsf[:, :, :], causT[:, :].unsqueeze(1).broadcast_to([P, 8, P]), ALU.mult),
              ["wsf", "causT"], ["WsT"])
            for g in range(8):
                A("pe", mm(ps[7][:, g:g + 1], WsT[:, g, :], onesb[:, 0:1], True, True), ["WsT", "onesb"], [("ps", 7)])
            A("dve", cp(rsum[:, :], ps[7][:, 0:8]), [("ps", 7)], ["rsum"])
            for g in range(8):
                A("dve", ts(B0[:, g * P:(g + 1) * P], LB[:, g * P:(g + 1) * P], rsum[:, g:g + 1], bsc[:, g:g + 1], ALU.mult, ALU.add),
                  ["LB", "rsum", "bsc"], ["B0"])

            def a_branch(tb):
                b = tb % 2
                s1 = st[:, 4 + b:5 + b]
                nm = st[:, 6 + b:7 + b]
                s2 = st[:, 8 + b:9 + b]
                A("dve", lambda e: e.tensor_reduce(out=s1, in_=Vr[:, tb, :], axis=AX.X, op=ALU.add), [("Vr", tb)], [("s1", b)])
                A("dve", ts(nm, s1, -1.0 / 1024, None, ALU.mult), [("s1", b)], [("nm", b)])
                A("dve", ts(Vr[:, tb, :], Vr[:, tb, :], nm, None, ALU.add), [("Vr", tb), ("nm", b)], [("Vr", tb)])
                A("dve", lambda e: e.memset(s2, 0.0), [], [("s2", b)])
                A("act", act_fn(junk1[:, :], Vr[:, tb, :], AF.Square, accum_out=s2), [("Vr", tb), ("s2", b)], ["junk1", ("s2", b)])
                r, rk = rstd_chain(s2, ("s2", b), 1024, b)
                A("dve", ts(vhat[0][:, :], Vr[:, tb, :], r, None, ALU.mult), [("Vr", tb), rk], [("vhat", 0)])
                pv = [5, 6]
                for g in range(8):
                    bank = pv[g // 4]
                    A("pe", mm(ps[bank][:, (g % 4) * P:(g % 4 + 1) * P], WsT[:, g, :], vhat[0][:, g * P:(g + 1) * P], True, True),
                      ["WsT", ("vhat", 0)], [("ps", bank)])
                for hf in range(2):
                    sl = slice(hf * 512, (hf + 1) * 512)
                    A("dve", tt(t1[0][:, sl], ps[pv[hf]][:, :], LG[:, sl], ALU.mult), [("ps", pv[hf]), "LG"], [("t1", 0, hf)])
                    A("dve", tt(t1[0][:, sl], t1[0][:, sl], B0[:, sl], ALU.add), [("t1", 0, hf), "B0"], [("t1", 0, hf)])
                    A("dve", tt(ya[0][:, sl], t1[0][:, sl], U[:, tb, sl], ALU.mult), [("t1", 0, hf), ("U", tb)], [("ya", 0)])
                pt = psbf(7)
                for j in range(8):
                    A("pe", tr(pt[:, j * P:(j + 1) * P], ya[0][:, j * P:(j + 1) * P]), [("ya", 0), "ident"], [("ps", 7)])
                A("act", lambda e: e.activation(out=yaT[:, :, tb * P:(tb + 1) * P], in_=pt.rearrange("p (j t) -> p j t", j=8), func=AF.Copy),
                  [("ps", 7)], ["yaT"])

            cnt = 0
            for ct in range(4):
                wt, wk = load_w(ring, ct * 512, 512, "wr")
                for tb in range(TB):
                    pbk = cnt % 4
                    b = cnt % 2
                    cnt += 1
                    proj_tok(wt, wk, 512, tb, pbk)
                    pp = ps[pbk][:, :]
                    A("act", act_fn(x2[0][:, :], pp, AF.Square), [("ps", pbk)], [("x2", 0)])
                    A("dve", stt(inn[0][:, :], x2[0][:, :], 1.0 / 0.044715, pp, ALU.add, ALU.mult), [("x2", 0), ("ps", pbk)], [("inn", 0)])
                    A("act", act_fn(sg[0][:, :], inn[0][:, :], AF.Sigmoid, scale=1.5957691216 * 0.044715), [("inn", 0)], [("sg", 0)])
                    if ct < 2:
                        dst, dk = U[:, tb, ct * 512:(ct + 1) * 512], ("U", tb)
                    else:
                        dst, dk = Vr[:, tb, (ct - 2) * 512:(ct - 1) * 512], ("Vr", tb)
                    A("dve", tt(dst, sg[0][:, :], pp, ALU.mult), [("sg", 0), ("ps", pbk)], [dk])
                    if ct == 3:
                        a_branch(tb)
            S.dma(Q, yaT_sp, yaT[:, :, :].rearrange("p j t -> p (j t)"), ["yaT"], ["yaT_sp"], "c5")
            fence(["yaT_sp"])
            DUMP("yaT", yaT[:, :, :], ["yaT"], BF16)
            DFENCE()
            S.emit()
            if STOP == "A1":
                return nc

        with ExitStack() as es:
            ring = [AL(es, "wr%d" % i, [P, 16, 512], BF16) for i in range(2)]
            QL = AL(es, "QL", [P, TB, 512], F32)
            qn = [AL(es, "qn%d" % i, [P, 512], BF16) for i in range(2)]
            qlT = AL(es, "qlT", [P, 4, TOK], BF16)
            wq = AL(es, "wq", [P, 4, 1024], BF16)
            wqi = AL(es, "wqi", [P, 4, 1024], BF16)
            C2 = AL(es, "C2", [P, TB, 128], F32)
            S2 = AL(es, "S2", [P, TB, 128], F32)
            Ci2 = AL(es, "Ci2", [P, TB, 32], F32)
            Si2 = AL(es, "Si2", [P, TB, 32], F32)
            qgc = AL(es, "qgc", [P, 4], F32)
            ta = [AL(es, "ta%d" % i, [P, 1024], F32) for i in range(2)]
            tb_ = [AL(es, "tbb%d" % i, [P, 1024], F32) for i in range(2)]
            qr = [AL(es, "qr%d" % i, [P, 1024], BF16) for i in range(2)]
            qi = [AL(es, "qi%d" % i, [P, 16, 64], F32) for i in range(2)]
            ua = [AL(es, "ua%d" % i, [P, 16, 32], F32) for i in range(2)]
            ub = [AL(es, "ub%d" % i, [P, 16, 32], F32) for i in range(2)]
            qib = [AL(es, "qib%d" % i, [P, 16, 64], BF16) for i in range(2)]
            junk2 = AL(es, "junk2", [P, 512], BF16)

            S.dma("pool", wq[:, :, :], wq_d.rearrange("(k p) n -> p k n", p=P), [], ["wq"], "c0")
            S.dma("pool", wqi[:, :, :], wqi_d.rearrange("(k p) n -> p k n", p=P), [], ["wqi"], "c1")
            S.dma(Q, C2[:, :, :], c2_d, [], ["C2"], "c2")
            S.dma(Q, S2[:, :, :], s2_d, [], ["S2"], "c3")
            S.dma(Q, Ci2[:, :, :], ci2_d, [], ["Ci2"], "c4")
            S.dma(Q, Si2[:, :, :], si2_d, [], ["Si2"], "c5")
            S.dma(Q, qgc[:, :], qg_d, [], ["qgc"], "c6")
            wt, wk = load_w(ring, COL_QLAT, 512, "wr")
            for tb in range(TB):
                pbk = tb % 4
                proj_tok(wt, wk, 512, tb, pbk)
                A("dve" if tb % 2 else "act", cp(QL[:, tb, :], ps[pbk][:, :]) if tb % 2 else act_fn(QL[:, tb, :], ps[pbk][:, :], AF.Copy),
                  [("ps", pbk)], [("QL", tb)])
            wt, wk = load_w(ring, COL_KIDX, 80, "wr")
            for tb in range(TB):
                pbk = tb % 4
                proj_tok(wt, wk, 80, tb, pbk)
                A("dve", cp(KI[:, tb, :], ps[pbk][:, 0:80]), [("ps", pbk)], [("KI", tb)])
            for tb in range(TB):
                b = tb % 2
                A("act", act_fn(wabs[:, tb, :], KI[:, tb, 64:80], AF.Abs, scale=IDXW_SCALE), [("KI", tb)], [("wabs", tb)])
                A("act", act_fn(sgn[:, tb, :], KI[:, tb, 64:80], AF.Sign), [("KI", tb)], [("sgn", tb)])
                ssq = st[:, b:b + 1]
                A("dve", lambda e, o=ssq: e.memset(o, 0.0), [], [("ssq", b)])
                A("act", act_fn(junk2[:, :], QL[:, tb, :], AF.Square, accum_out=ssq), [("QL", tb), ("ssq", b)], ["junk2", ("ssq", b)])
                r, rk = rstd_chain(ssq, ("ssq", b), 512, b)
                A("dve", ts(qn[b][:, :], QL[:, tb, :], r, None, ALU.mult), [("QL", tb), rk], [("qn", b)])
                pt = psbf(4 + b)
                for k in range(4):
                    A("pe", tr(pt[:, k * P:(k + 1) * P], qn[b][:, k * P:(k + 1) * P]), [("qn", b), "ident"], [("ps", 4 + b)])
                for k in range(4):
                    A("dve", ts(qlT[:, k, tb * P:(tb + 1) * P], pt[:, k * P:(k + 1) * P], qgc[:, k:k + 1], None, ALU.mult),
                      [("ps", 4 + b), "qgc"], [("qlT", tb)])
                for n in range(2):
                    for k in range(4):
                        A("pe", mm(ps[n][:, :], qlT[:, k, tb * P:(tb + 1) * P], wq[:, k, n * 512:(n + 1) * 512], k == 0, k == 3),
                          [("qlT", tb), "wq"], [("ps", n)])
                for n in range(2):
                    pq = ps[n][:, :].rearrange("p (h f) -> p h f", h=4)
                    tav = ta[b][:, n * 512:(n + 1) * 512].rearrange("p (h f) -> p h f", h=4)
                    tbv = tb_[b][:, n * 512:(n + 1) * 512].rearrange("p (h f) -> p h f", h=4)
                    qrv = qr[b][:, n * 512:(n + 1) * 512].rearrange("p (h f) -> p h f", h=4)
                    c2b = C2[:, tb, :].unsqueeze(1).broadcast_to([P, 4, 128])
                    A("dve", tt(tav, pq, c2b, ALU.mult), [("ps", n), "C2"], [("ta", b, n)])
                    A("dve", tt(tbv[:, :, 0:64], pq[:, :, 64:128], S2[:, tb, 0:64].unsqueeze(1).broadcast_to([P, 4, 64]), ALU.mult),
                      [("ps", n), "S2"], [("tb", b, n)])
                    A("dve", tt(tbv[:, :, 64:128], pq[:, :, 0:64], S2[:, tb, 64:128].unsqueeze(1).broadcast_to([P, 4, 64]), ALU.mult),
                      [("ps", n), "S2"], [("tb", b, n)])
                    A("dve", tt(qrv, tav, tbv, ALU.add), [("ta", b, n), ("tb", b, n)], [("qr", b)])
                pt2 = psbf(6 + b)
                for h in range(8):
                    A("pe", tr(pt2[:, h * P:(h + 1) * P], qr[b][:, h * P:(h + 1) * P]), [("qr", b), "ident"], [("ps", 6 + b)])
                A("act", lambda e, tb=tb, pt2=pt2: e.activation(out=qT[:, tb, :, :], in_=pt2.rearrange("p (h t) -> p h t", h=8), func=AF.Copy),
                  [("ps", 6 + b)], [("qT", tb)])
                for n in range(2):
                    for k in range(4):
                        A("pe", mm(ps[2 + n][:, :], qlT[:, k, tb * P:(tb + 1) * P], wqi[:, k, n * 512:(n + 1) * 512], k == 0, k == 3),
                          [("qlT", tb), "wqi"], [("ps", 2 + n)])
                for n in range(2):
                    pq = ps[2 + n][:, :].rearrange("p (h f) -> p h f", h=8)
                    A("dve", tt(qi[b][:, 8 * n:8 * n + 8, :], pq, wabs[:, tb, 8 * n:8 * n + 8].unsqueeze(2).broadcast_to([P, 8, 64]), ALU.mult),
                      [("ps", 2 + n), ("wabs", tb)], [("qi", b)])
                A("dve", tt(ua[b][:, :, :], qi[b][:, :, 0:32], Ci2[:, tb, :].unsqueeze(1).broadcast_to([P, 16, 32]), ALU.mult),
                  [("qi", b), "Ci2"], [("ua", b)])
                A("dve", tt(ub[b][:, :, 0:16], qi[b][:, :, 16:32], Si2[:, tb, 0:16].unsqueeze(1).broadcast_to([P, 16, 16]), ALU.mult),
                  [("qi", b), "Si2"], [("ub", b)])
                A("dve", tt(ub[b][:, :, 16:32], qi[b][:, :, 0:16], Si2[:, tb, 16:32].unsqueeze(1).broadcast_to([P, 16, 16]), ALU.mult),
                  [("qi", b), "Si2"], [("ub", b)])
                A("dve", tt(qib[b][:, :, 0:32], ua[b][:, :, :], ub[b][:, :, :], ALU.add), [("ua", b), ("ub", b)], [("qib", b)])
                A("dve", cp(qib[b][:, :, 32:64], qi[b][:, :, 32:64]), [("qi", b)], [("qib", b)])
                pt3 = psbf(4 + b)
                qibf = qib[b][:, :, :].rearrange("p h f -> p (h f)")
                for j in range(8):
                    A("pe", tr(pt3[:, j * P:(j + 1) * P], qibf[:, j * P:(j + 1) * P]), [("qib", b), "ident"], [("ps", 4 + b)])
                A("act", lambda e, tb=tb, pt3=pt3: e.activation(out=qiT[:, tb, :, :], in_=pt3.rearrange("p (h t) -> p h t", h=8), func=AF.Copy),
                  [("ps", 4 + b)], [("qiT", tb)])
            DUMP("qT", qT[:, :, :, :], [("qT", t_) for t_ in range(TB)], BF16)
            DUMP("qiT", qiT[:, :, :, :], [("qiT", t_) for t_ in range(TB)], BF16)
            DUMP("sgn", sgn[:, :, :], [("sgn", t_) for t_ in range(TB)])
            DUMP("wabs", wabs[:, :, :], [("wabs", t_) for t_ in range(TB)])
            DFENCE()
            S.emit()
            if STOP == "A2":
                return nc

        with ExitStack() as es:
            ring = [AL(es, "wr%d" % i, [P, 16, 512], BF16) for i in range(3)]
            KTl = AL(es, "KTl", [P, 8, TOK], BF16)
            Vx = AL(es, "Vx", [P, TB, 8, 129], BF16)
            kT64 = AL(es, "kT64", [P, TOK], BF16)
            C2 = AL(es, "C2", [P, TB, 128], F32)
            S2 = AL(es, "S2", [P, TB, 128], F32)
            Ci2 = AL(es, "Ci2", [P, TB, 32], F32)
            Si2 = AL(es, "Si2", [P, TB, 32], F32)
            KG = AL(es, "KG", [P, 64], F32)
            KB = AL(es, "KB", [P, 64], F32)
            ta = [AL(es, "ta%d" % i, [P, 512], F32) for i in range(2)]
            tb_ = [AL(es, "tbb%d" % i, [P, 512], F32) for i in range(2)]
            kr = [AL(es, "kr%d" % i, [P, 512], BF16) for i in range(2)]
            kn = [AL(es, "kn%d" % i, [P, 64], F32) for i in range(2)]
            ka = [AL(es, "ka%d" % i, [P, 32], F32) for i in range(2)]
            kb = [AL(es, "kb%d" % i, [P, 32], F32) for i in range(2)]
            kib = [AL(es, "kib%d" % i, [P, 64], BF16) for i in range(2)]
            junk3 = AL(es, "junk3", [P, 64], BF16)

            S.dma(Q, C2[:, :, :], c2_d, [], ["C2"], "c2")
            S.dma(Q, S2[:, :, :], s2_d, [], ["S2"], "c3")
            S.dma(Q, Ci2[:, :, :], ci2_d, [], ["Ci2"], "c4")
            S.dma(Q, Si2[:, :, :], si2_d, [], ["Si2"], "c5")
            S.dma(Q, KG[:, :], bcast_row2(kig_d), [], ["KG"], "c6")
            S.dma(Q, KB[:, :], bcast_row2(kib_d), [], ["KB"], "c7")
            A("dve", lambda e: e.memset(Vx[:, :, :, 128:129], 1.0), [], ["Vx1"])
            cnt = 0
            for n in range(2):
                wt, wk = load_w(ring, COL_K + n * 512, 512, "wr")
                for tb in range(TB):
                    pbk = cnt % 4
                    b = cnt % 2
                    cnt += 1
                    proj_tok(wt, wk, 512, tb, pbk)
                    pq = ps[pbk][:, :].rearrange("p (h f) -> p h f", h=4)
                    tav = ta[b][:, :].rearrange("p (h f) -> p h f", h=4)
                    tbv = tb_[b][:, :].rearrange("p (h f) -> p h f", h=4)
                    krv = kr[b][:, :].rearrange("p (h f) -> p h f", h=4)
                    A("dve", tt(tav, pq, C2[:, tb, :].unsqueeze(1).broadcast_to([P, 4, 128]), ALU.mult), [("ps", pbk), "C2"], [("ta", b)])
                    A("dve", tt(tbv[:, :, 0:64], pq[:, :, 64:128], S2[:, tb, 0:64].unsqueeze(1).broadcast_to([P, 4, 64]), ALU.mult),
                      [("ps", pbk), "S2"], [("tb", b)])
                    A("dve", tt(tbv[:, :, 64:128], pq[:, :, 0:64], S2[:, tb, 64:128].unsqueeze(1).broadcast_to([P, 4, 64]), ALU.mult),
                      [("ps", pbk), "S2"], [("tb", b)])
                    A("dve", tt(krv, tav, tbv, ALU.add), [("ta", b), ("tb", b)], [("kr", b)])
                    pt = psbf(4 + b)
                    for h in range(4):
                        A("pe", tr(pt[:, h * P:(h + 1) * P], kr[b][:, h * P:(h + 1) * P]), [("kr", b), "ident"], [("ps", 4 + b)])
                    A("act", lambda e, n=n, tb=tb, pt=pt: e.activation(out=KTl[:, 4 * n:4 * n + 4, tb * P:(tb + 1) * P],
                                                                      in_=pt[:, 0:512].rearrange("p (h t) -> p h t", h=4), func=AF.Copy),
                      [("ps", 4 + b)], ["KTl"])
            for n in range(2):
                wt, wk = load_w(ring, COL_V + n * 512, 512, "wr")
                for tb in range(TB):
                    pbk = cnt % 4
                    cnt += 1
                    proj_tok(wt, wk, 512, tb, pbk)
                    A("act", lambda e, n=n, tb=tb, pbk=pbk: e.activation(out=Vx[:, tb, 4 * n:4 * n + 4, 0:128],
                                                                        in_=ps[pbk][:, :].rearrange("p (h f) -> p h f", h=4), func=AF.Copy),
                      [("ps", pbk)], ["Vx"])
            for tb in range(TB):
                b = tb % 2
                s1 = st[:, 4 + b:5 + b]
                nm = st[:, 6 + b:7 + b]
                s2 = st[:, 8 + b:9 + b]
                A("dve", lambda e, s1=s1, tb=tb: e.tensor_reduce(out=s1, in_=KI[:, tb, 0:64], axis=AX.X, op=ALU.add), [("KI", tb)], [("s1", b)])
                A("dve", ts(nm, s1, -1.0 / 64, None, ALU.mult), [("s1", b)], [("nm", b)])
                A("dve", ts(kn[b][:, :], KI[:, tb, 0:64], nm, None, ALU.add), [("KI", tb), ("nm", b)], [("kn", b)])
                A("dve", lambda e, s2=s2: e.memset(s2, 0.0), [], [("s2", b)])
                A("act", act_fn(junk3[:, :], kn[b][:, :], AF.Square, accum_out=s2), [("kn", b), ("s2", b)], ["junk3", ("s2", b)])
                r, rk = rstd_chain(s2, ("s2", b), 64, b)
                A("dve", ts(kn[b][:, :], kn[b][:, :], r, None, ALU.mult), [("kn", b), rk], [("kn", b)])
                A("dve", tt(kn[b][:, :], kn[b][:, :], KG[:, :], ALU.mult), [("kn", b), "KG"], [("kn", b)])
                A("dve", tt(kn[b][:, :], kn[b][:, :], KB[:, :], ALU.add), [("kn", b), "KB"], [("kn", b)])
                A("dve", tt(ka[b][:, :], kn[b][:, 0:32], Ci2[:, tb, :], ALU.mult), [("kn", b), "Ci2"], [("ka", b)])
                A("dve", tt(kb[b][:, 0:16], kn[b][:, 16:32], Si2[:, tb, 0:16], ALU.mult), [("kn", b), "Si2"], [("kb", b)])
                A("dve", tt(kb[b][:, 16:32], kn[b][:, 0:16], Si2[:, tb, 16:32], ALU.mult), [("kn", b), "Si2"], [("kb", b)])
                A("dve", tt(kib[b][:, 0:32], ka[b][:, :], kb[b][:, :], ALU.add), [("ka", b), ("kb", b)], [("kib", b)])
                A("dve", cp(kib[b][:, 32:64], kn[b][:, 32:64]), [("kn", b)], [("kib", b)])
                pt = psbf(6 + b)
                A("pe", tr(pt[0:64, 0:P], kib[b][:, :]), [("kib", b), "ident"], [("ps", 6 + b)])
                A("act", act_fn(kT64[0:64, tb * P:(tb + 1) * P], pt[0:64, 0:P], AF.Copy), [("ps", 6 + b)], ["kT64"])
            S.dma(Q, kt_in.rearrange("(h d) t -> d h t", h=8), KTl[:, :, :], ["KTl"], ["kt_in"], "c8")
            S.dma(Q, v_in.rearrange("(b s) f -> s b f", b=8), Vx[:, :, :, :].rearrange("p b h f -> p b (h f)"), ["Vx", "Vx1"], ["v_in"], "c9")
            S.dma(Q, ki_in, kT64[0:64, :], ["kT64"], ["ki_in"], "c10")
            S.dma(Q, hT_sp, hT[:, :, :].rearrange("p k t -> p (k t)"), HT_ALL, ["hT_sp"], "c11")
            for nm_, a_in, a_out in (("kt", kt_in, kt_all), ("v", v_in, v_all), ("ki", ki_in, ki_all)):
                A("pool", lambda e, a_in=a_in, a_out=a_out: e.collective_compute(
                    "AllGather", ALU.bypass, replica_groups=[list(range(NC))], ins=[a_in], outs=[a_out]),
                  [nm_ + "_in"], [nm_ + "_all"], kind="cc")
            fence(["kt_all", "v_all", "ki_all", "hT_sp"])
            DUMP("KTl", KTl[:, :, :], ["KTl"], BF16)
            DUMP("Vx", Vx[:, :, :, :], ["Vx", "Vx1"], BF16)
            DUMP("kT64", kT64[0:64, :], ["kT64"], BF16)
            DFENCE()
            S.emit()
            if STOP == "A3":
                return nc

    U8 = mybir.dt.uint8
    with ExitStack() as es:
        kiT2 = AL(es, "kiT2", [P, 8, TOK], BF16)
        SCs = [AL(es, "SC0", [P, 7168], F32), AL(es, "SC1", [P, 8192], F32)]
        NMt = AL(es, "NM", [P, 8192], BF16)
        junk8 = AL(es, "junk8", [P, 8192], U8)
        MA = AL(es, "MA", [P, 8, P], F32)
        Kb = [AL(es, "Kb%d" % i, [P, 8, 512], BF16) for i in range(2)]
        Vb = [AL(es, "Vb%d" % i, [P, 4, 1032], BF16) for i in range(2)]
        Z = [AL(es, "Z%d" % i, [P, 512], BF16) for i in range(4)]
        PT = [AL(es, "PT%d" % i, [P, 1024], BF16) for i in range(2)]
        DSs = [AL(es, "DS%d" % i, [P, 16, P], BF16) for i in range(2)]
        yb = AL(es, "yb", [P, 1024], BF16)
        ybT = AL(es, "ybT", [P, 8, P], BF16)
        pw2 = AL(es, "pw2", [P, NIT + 1], F32)
        wtab = AL(es, "wtab", [P, NIT + 1], F32)
        bv = AL(es, "bv", [P, 16], F32)
        rec = AL(es, "rec", [P, 8], F32)
        lnd = AL(es, "lnd", [P, 8], F32)
        thrall = AL(es, "thrall", [P, 16], F32)

        kiv = ki_all.rearrange("(r d) t -> d r t", r=8)
        S.dma(Q, kiT2[0:64, :, :], kiv, [], ["kiT2a"], "c0")
        S.dma(Q, kiT2[64:128, :, :], kiv, [], ["kiT2b"], "c1")
        S.dma(Q, pw2[:, :], pow2_d, [], ["pw2"], "c2")
        ktv = kt_all.rearrange("(r h d) t -> d r h t", r=8, h=8)
        vv = v_all.rearrange("(r b s) f -> s r b f", r=8, b=8)
        ybTv = ybT_sp.rearrange("p (j t) -> p j t", j=8)
        kvc = [0]
        zc = [0]

        def chunks_of(lb):
            m = lb + 1
            out = []
            for r in range(8):
                b0_ = 0
                while b0_ < m:
                    nb = min(4, m - b0_)
                    out.append((r, b0_, nb))
                    b0_ += nb
            return out

        def stage_DS(lb):
            DS = DSs[lb % 2]
            for h in range(16):
                A("dve", ts(DS[:, h, :], ident[:, :], sgn[:, lb, h:h + 1], None, ALU.mult), ["ident", ("sgn", lb)], [("DS", lb % 2)])

        def stage_I(lb):
            m = lb + 1
            SC = SCs[lb % 2]
            sck = ("SC", lb % 2)
            DS = DSs[lb % 2]
            dsk = ("DS", lb % 2)
            for (r, b0_, nb) in chunks_of(lb):
                N = nb * P
                off = (r * m + b0_) * P
                psS = 2
                pend = []
                for h in range(16):
                    pl = h % 2
                    lo_, hi_ = (h % 2) * 64, (h % 2) * 64 + 64
                    A("pe", mm(ps[pl][:, 0:N], qiT[lo_:hi_, lb, h // 2, :], kiT2[lo_:hi_, r, b0_ * P:b0_ * P + N], True, True),
                      [("qiT", lb), "kiT2a", "kiT2b"], [("ps", pl)])
                    zi = zc[0] % 4
                    zc[0] += 1
                    A("act", act_fn(Z[zi][:, 0:N], ps[pl][:, 0:N], AF.Relu), [("ps", pl)], [("Z", zi)])
                    pend.append((h, zi))
                    if len(pend) > 1:
                        hh, zz = pend.pop(0)
                        A("pe", mm(ps[psS][:, 0:N], DS[:, hh, :], Z[zz][:, 0:N], hh == 0, hh == 15), [dsk, ("Z", zz)], [("ps", psS)])
                    yield
                for hh, zz in pend:
                    A("pe", mm(ps[psS][:, 0:N], DS[:, hh, :], Z[zz][:, 0:N], hh == 0, hh == 15), [dsk, ("Z", zz)], [("ps", psS)])
                A("act", act_fn(SC[:, off:off + N], ps[psS][:, 0:N], AF.Copy), [("ps", psS)], [sck])

        def stage_T(lb):
            m = lb + 1
            nk = 1024 * m
            SC = SCs[lb % 2]
            sck = ("SC", lb % 2)
            hi0 = bv[:, 0:1]
            lo0 = bv[:, 1:2]
            w0 = bv[:, 2:3]
            mid = bv[:, 3:4]
            cntv = bv[:, 4:5]
            dv = bv[:, 5:6]
            thr = bv[:, 6:7]
            A("dve", lambda e: e.tensor_reduce(out=hi0, in_=SC[:, 0:nk], axis=AX.X, op=ALU.max), [sck], ["hi0"])
            A("dve", lambda e: e.tensor_reduce(out=lo0, in_=SC[:, 0:nk], axis=AX.X, op=ALU.min), [sck], ["lo0"])
            A("dve", ts(lo0, lo0, -1.0, None, ALU.add), ["lo0"], ["lo0"])
            A("dve", tt(w0, hi0, lo0, ALU.subtract), ["hi0", "lo0"], ["w0"])
            A("dve", ts(wtab[:, :], pw2[:, :], w0, None, ALU.mult), ["pw2", "w0"], ["wtab"])
            A("dve", tt(mid, lo0, wtab[:, 0:1], ALU.add), ["lo0", "wtab"], ["mid"])
            scl = SC[:, 0:nk].rearrange("p (r m s) -> p r m s", r=8, m=m)[:, :, m - 1, :]
            A("dve", tt(scl, scl, MA[:, :, :], ALU.add), [sck, "MA"], [sck])
            for it in range(NIT):
                A("dve", lambda e: e.tensor_scalar(out=junk8[:, 0:nk], in0=SC[:, 0:nk], scalar1=mid, scalar2=None,
                                                   op0=ALU.is_ge, op1=ALU.add, accum_out=cntv),
                  [sck, "mid"], ["junk8", "cnt"])
                A("dve", ts(dv, cntv, 256.0, 0.5, ALU.is_ge, ALU.subtract), ["cnt"], ["dv"])
                A("dve", stt(mid, dv, wtab[:, it:it + 1], mid, ALU.mult, ALU.add), ["dv", "wtab", "mid"], ["mid"])
            A("dve", tt(thr, mid, wtab[:, NIT:NIT + 1], ALU.subtract), ["mid", "wtab"], ["thr"])
            A("dve", ts(NMt[:, 0:nk], SC[:, 0:nk], thr, None, ALU.is_lt), [sck, "thr"], ["NM"])
            if debug:
                A("dve", cp(thrall[:, lb:lb + 1], thr), ["thr"], ["thrall"])
                A("dve", cp(thrall[:, 8 + lb:9 + lb], cntv), ["cnt"], ["thrall"])

        def stage_A(lb):
            m = lb + 1
            ntile = 8 * m
            ti = 0
            for (r, b0_, nb) in chunks_of(lb):
                kb_i = kvc[0] % 2
                kvc[0] += 1
                S.dma(Q, Kb[kb_i][:, :, 0:nb * P], ktv[:, r, :, b0_ * P:(b0_ + nb) * P], [], [("Kb", kb_i)], ("Kb", kb_i))
                S.dma(Q, Vb[kb_i][:, 0:nb, :], vv[:, r, b0_:b0_ + nb, :], [], [("Vb", kb_i)], ("Vb", kb_i))
                for j in range(nb):
                    off = (r * m + b0_ + j) * P
                    pti = ti % 2
                    for hg in range(2):
                        pa = 3 + hg
                        A("pe", mm(ps[pa][:, :], NMt[:, off:off + P], negi4[:, :], True, False), ["NM", "negi4"], [("ps", pa)])
                        for hh in range(4):
                            h = hg * 4 + hh
                            A("pe", mm(ps[pa][:, hh * P:(hh + 1) * P], Kb[kb_i][:, h, j * P:(j + 1) * P], qT[:, lb, h, :], False, hh == 3),
                              [("Kb", kb_i), ("qT", lb)], [("ps", pa)])
                        A("act", act_fn(PT[pti][:, hg * 512:(hg + 1) * 512], ps[pa][:, :], AF.Exp, scale=ATT_SCALE),
                          [("ps", pa)], [("PT", pti, hg)])
                        yield
                    for h in range(8):
                        bank = 5 + h // 3
                        c0 = (h % 3) * 129
                        A("pe", mm(ps[bank][:, c0:c0 + 129], PT[pti][:, h * P:(h + 1) * P], Vb[kb_i][:, j, h * 129:(h + 1) * 129],
                                   ti == 0 and h % 3 == 0, ti == ntile - 1),
                          [("PT", pti, h // 4), ("Vb", kb_i)], [("ps", bank)])
                    ti += 1
                    yield
            for h in range(8):
                bank = 5 + h // 3
                c0 = (h % 3) * 129
                A("act", act_fn(lnd[:, h:h + 1], ps[bank][:, c0 + 128:c0 + 129], AF.Ln), [("ps", bank)], ["lnd"])
            A("act", act_fn(rec[:, :], lnd[:, :], AF.Exp, scale=-1.0), ["lnd"], ["rec"])
            for h in range(8):
                bank = 5 + h // 3
                c0 = (h % 3) * 129
                A("act", act_fn(yb[:, h * P:(h + 1) * P], ps[bank][:, c0:c0 + 128], AF.Copy, scale=rec[:, h:h + 1]),
                  [("ps", bank), "rec"], ["yb"])
            pt = psbf(3)
            for j in range(8):
                A("pe", tr(pt[:, j * P:(j + 1) * P], yb[:, j * P:(j + 1) * P]), ["yb", "ident"], [("ps", 3)])
            A("act", lambda e, pt=pt: e.activation(out=ybT[:, :, :], in_=pt.rearrange("p (j t) -> p j t", j=8), func=AF.Copy),
              [("ps", 3)], ["ybT"])
            S.dma(Q, ybTv[:, :, lb * P:(lb + 1) * P], ybT[:, :, :], ["ybT"], ["ybT_sp"], "c4")
            DUMP("ybT%d" % lb, ybT[:, :, :], ["ybT"], BF16)

        def merged(gens):
            live = [[g, float(n), 0] for g, n in gens]
            while live:
                live.sort(key=lambda x: x[2] / x[1])
                it = live[0]
                try:
                    next(it[0])
                    it[2] += 1
                except StopIteration:
                    live.remove(it)

        stage_DS(0)
        merged([(stage_I(0), 1)])
        for lb in range(TB):
            if lb + 1 < TB:
                stage_DS(lb + 1)
            S.dma(Q, MA[:, :, :], madd_d[lb], [], ["MA"], "c3")
            gens = []
            if lb >= 1:
                gens.append((stage_A(lb - 1), 3 * 8 * lb + 1))
            if lb + 1 < TB:
                gens.append((stage_I(lb + 1), 16 * len(chunks_of(lb + 1)) + 1))
            merged(gens)
            stage_T(lb)
        merged([(stage_A(TB - 1), 1)])
        fence(["ybT_sp"])
        DUMP("thr", thrall[:, :], ["thrall"])
        DFENCE()
        S.emit()
        if STOP == "B":
            return nc
    esAB.close()

    with ExitStack() as esC:
        mT = AL(esC, "mT", [P, 16, TOK], BF16)
        with ExitStack() as es:
            hT = AL(es, "hT2", [P, 16, TOK], BF16)
            yaT = AL(es, "yaT2", [P, 8, TOK], BF16)
            ybT = AL(es, "ybT2", [P, 8, TOK], BF16)
            wga = [AL(es, "wga%d" % i, [P, 16, 256], BF16) for i in range(2)]
            wgb = [AL(es, "wgb%d" % i, [P, 16, 256], BF16) for i in range(2)]
            wpa = [AL(es, "wpa%d" % i, [P, 8, 256], BF16) for i in range(2)]
            wpb = [AL(es, "wpb%d" % i, [P, 8, 256], BF16) for i in range(2)]
            sa = [AL(es, "sa%d" % i, [P, 512], BF16) for i in range(2)]
            sbb = [AL(es, "sbb%d" % i, [P, 512], BF16) for i in range(2)]
            m1 = [AL(es, "m1_%d" % i, [P, 512], F32) for i in range(2)]
            m2 = [AL(es, "m2_%d" % i, [P, 512], F32) for i in range(2)]
            S.dma(Q, hT[:, :, :].rearrange("p k t -> p (k t)"), hT_sp, [], ["hT"], "c0")
            S.dma(Q, yaT[:, :, :].rearrange("p k t -> p (k t)"), yaT_sp, [], ["yaT"], "c1")
            S.dma(Q, ybT[:, :, :].rearrange("p k t -> p (k t)"), ybT_sp, [], ["ybT"], "c2")
            wpav = wpa_d.rearrange("(k p) n -> p k n", p=P)
            wpbv = wpb_d.rearrange("(k p) n -> p k n", p=P)
            cnt = 0
            for jg in range(8):
                s = jg % 2
                c0 = jg * 256
                S.dma("pool", wga[s][:, :, :], winv[:, :, COL_GATE + c0:COL_GATE + c0 + 256], [], [("wga", s)], ("wga", s))
                S.dma("pool", wgb[s][:, :, :], winv[:, :, COL_GATE + D + c0:COL_GATE + D + c0 + 256], [], [("wgb", s)], ("wgb", s))
                S.dma("pool", wpa[s][:, :, :], wpav[:, :, c0:c0 + 256], [], [("wpa", s)], ("wpa", s))
                S.dma("pool", wpb[s][:, :, :], wpbv[:, :, c0:c0 + 256], [], [("wpb", s)], ("wpb", s))
                for jj in range(2):
                    j = jg * 2 + jj
                    cs = slice(jj * P, (jj + 1) * P)
                    for hf in range(2):
                        tsl = slice(hf * 512, (hf + 1) * 512)
                        pb0 = (cnt % 2) * 4
                        b = cnt % 2
                        cnt += 1
                        for k in range(16):
                            A("pe", mm(ps[pb0][:, :], wga[s][:, k, cs], hT[:, k, tsl], k == 0, k == 15), [("wga", s), "hT"], [("ps", pb0)])
                        for k in range(16):
                            A("pe", mm(ps[pb0 + 1][:, :], wgb[s][:, k, cs], hT[:, k, tsl], k == 0, k == 15), [("wgb", s), "hT"], [("ps", pb0 + 1)])
                        for k in range(8):
                            A("pe", mm(ps[pb0 + 2][:, :], wpa[s][:, k, cs], yaT[:, k, tsl], k == 0, k == 7), [("wpa", s), "yaT"], [("ps", pb0 + 2)])
                        for k in range(8):
                            A("pe", mm(ps[pb0 + 3][:, :], wpb[s][:, k, cs], ybT[:, k, tsl], k == 0, k == 7), [("wpb", s), "ybT"], [("ps", pb0 + 3)])
                        A("act", act_fn(sa[b][:, :], ps[pb0][:, :], AF.Sigmoid), [("ps", pb0)], [("sa", b)])
                        A("act", act_fn(sbb[b][:, :], ps[pb0 + 1][:, :], AF.Sigmoid), [("ps", pb0 + 1)], [("sbb", b)])
                        A("dve", tt(m1[b][:, :], sa[b][:, :], ps[pb0 + 2][:, :], ALU.mult), [("sa", b), ("ps", pb0 + 2)], [("m1", b)])
                        A("dve", tt(m2[b][:, :], sbb[b][:, :], ps[pb0 + 3][:, :], ALU.mult), [("sbb", b), ("ps", pb0 + 3)], [("m2", b)])
                        A("dve", tt(mT[:, j, tsl], m1[b][:, :], m2[b][:, :], ALU.add), [("m1", b), ("m2", b)], ["mT"])
            DUMP("mT", mT[:, :, :], ["mT"], BF16)
            DFENCE()
            S.emit()
            if STOP == "C1":
                return nc
        h2T = nc.alloc_sbuf_tensor_at("h2T", [P, 16, TOK], BF16, offset=196576)
        with ExitStack() as es:
            wo = AL(es, "wo", [P, 16, D], BF16)
            wov = wo_d.rearrange("(k p) n -> p k n", p=P)
            for g in range(4):
                S.dma("pool", wo[:, :, g * 512:(g + 1) * 512], wov[:, :, g * 512:(g + 1) * 512], [], [("wo", g)], ("wo", g))
            xs = [AL(es, "xs%d" % i, [P, D], F32) for i in range(2)]
            tt_ = [AL(es, "tt%d" % i, [P, D], F32) for i in range(2)]
            xn2 = [AL(es, "xn2%d" % i, [P, D], BF16) for i in range(2)]
            gmb = AL(es, "gmb2", [P, D], F32)
            junk = AL(es, "junkc", [P, D], BF16)
            S.dma(Q, gmb[:, :], gmb_sp, [], ["gmb"], "c0")
            for tb in range(TB):
                b = tb % 2
                S.dma(Q, xs[b][:, :], x_d[tb * P:(tb + 1) * P, :], [], [("xs", b)], ("xs", b))
                pb0 = b * 4
                for cg in range(4):
                    for k in range(16):
                        A("pe", mm(ps[pb0 + cg][:, :], mT[:, k, tb * P:(tb + 1) * P], wo[:, k, cg * 512:(cg + 1) * 512], k == 0, k == 15),
                          [("wo", cg)], [("ps", pb0 + cg)])
                sq4 = st[:, 12 + 4 * b:16 + 4 * b]
                A("dve", lambda e, o=sq4: e.memset(o, 0.0), [], [("sq4", b)])
                for cg in range(4):
                    A("act", act_fn(junk[:, cg * 512:(cg + 1) * 512], ps[pb0 + cg][:, :], AF.Square, accum_out=sq4[:, cg:cg + 1]),
                      [("ps", pb0 + cg), ("sq4", b)], ["junk", ("sq4", b)])
                ssq = st[:, b:b + 1]
                A("dve", lambda e, o=ssq, i=sq4: e.tensor_reduce(out=o, in_=i, axis=AX.X, op=ALU.add), [("sq4", b)], [("ssq", b)])
                r, rk = rstd_chain(ssq, ("ssq", b), D, b)
                for cg in range(4):
                    sl = slice(cg * 512, (cg + 1) * 512)
                    A("dve", ts(tt_[b][:, sl], ps[pb0 + cg][:, :], r, None, ALU.mult), [("ps", pb0 + cg), rk], [("tt", b, cg)])
                    A("dve", tt(tt_[b][:, sl], tt_[b][:, sl], gmb[:, sl], ALU.mult), [("tt", b, cg), "gmb"], [("tt", b, cg)])
                    A("dve", tt(xs[b][:, sl], xs[b][:, sl], tt_[b][:, sl], ALU.add), [("tt", b, cg), ("xs", b)], [("xs", b)])
                S.dma(Q, x1_sp[tb * P:(tb + 1) * P, :], xs[b][:, :], [("xs", b)], ["x1_sp"], ("x1s", b))
                ssq2 = st[:, 2 + b:3 + b]
                A("dve", lambda e, o=ssq2: e.memset(o, 0.0), [], [("ssq2", b)])
                A("act", act_fn(junk[:, :], xs[b][:, :], AF.Square, accum_out=ssq2), [("xs", b), ("ssq2", b)], ["junk", ("ssq2", b)])
                r2, rk2 = rstd_chain(ssq2, ("ssq2", b), D, b)
                A("dve", ts(xn2[b][:, :], xs[b][:, :], r2, None, ALU.mult), [("xs", b), rk2], [("xn2", b)])
                for g4 in range(4):
                    pbk = (1 - b) * 4 + g4
                    pt = psbf(pbk)
                    for i in range(4):
                        k = g4 * 4 + i
                        A("pe", tr(pt[:, i * P:(i + 1) * P], xn2[b][:, k * P:(k + 1) * P]), [("xn2", b), "ident"], [("ps", pbk)])
                    for i in range(4):
                        k = g4 * 4 + i
                        o = h2T[:, k, tb * P:(tb + 1) * P]
                        if i % 2 == 0:
                            A("dve", ts(o, pt[:, i * P:(i + 1) * P], gsf_c[:, k:k + 1], shf_c[:, k:k + 1], ALU.mult, ALU.add),
                              [("ps", pbk)], ["h2T"])
                        else:
                            A("act", act_fn(o, pt[:, i * P:(i + 1) * P], AF.Identity, scale=gsf_c[:, k:k + 1], bias=shf_c[:, k:k + 1]),
                              [("ps", pbk)], ["h2T"])
            fence(["x1_sp"])
            DUMP("h2T", h2T[:, :, :], ["h2T"], BF16)
            DFENCE()
            S.emit()
            if STOP == "C2":
                return nc

    with ExitStack() as esF:
        aT = AL(esF, "aT", [P, 44, TOK], BF16)
        with ExitStack() as es:
            wG = [AL(es, "wG%d" % i, [P, 16, 256], BF16) for i in range(2)]
            wU = [AL(es, "wU%d" % i, [P, 16, 256], BF16) for i in range(2)]
            sgl = [AL(es, "sgl%d" % i, [P, 512], F32) for i in range(2)]
            wgv = wg_d.rearrange("(k p) n -> p k n", p=P)
            wuv = wu_d.rearrange("(k p) n -> p k n", p=P)
            cnt = 0
            for jg in range(22):
                s = jg % 2
                S.dma("pool", wG[s][:, :, :], wgv[:, :, jg * 256:(jg + 1) * 256], [], [("wG", s)], ("wG", s))
                S.dma("pool", wU[s][:, :, :], wuv[:, :, jg * 256:(jg + 1) * 256], [], [("wU", s)], ("wU", s))
                for jj in range(2):
                    j = jg * 2 + jj
                    cs = slice(jj * P, (jj + 1) * P)
                    for hf in range(2):
                        tsl = slice(hf * 512, (hf + 1) * 512)
                        pg = (cnt % 4) * 2
                        b = cnt % 2
                        cnt += 1
                        for k in range(16):
                            A("pe", mm(ps[pg][:, :], wG[s][:, k, cs], h2T[:, k, tsl], k == 0, k == 15), [("wG", s)], [("ps", pg)])
                        for k in range(16):
                            A("pe", mm(ps[pg + 1][:, :], wU[s][:, k, cs], h2T[:, k, tsl], k == 0, k == 15), [("wU", s)], [("ps", pg + 1)])
                        A("act", act_fn(sgl[b][:, :], ps[pg][:, :], AF.Silu), [("ps", pg)], [("sgl", b)])
                        A("dve", tt(aT[:, j, tsl], sgl[b][:, :], ps[pg + 1][:, :], ALU.mult), [("sgl", b), ("ps", pg + 1)], ["aT"])
            S.emit()
            if STOP == "D":
                return nc
        with ExitStack() as es:
            wd = [AL(es, "wd%d" % i, [P, 4, 512], BF16) for i in range(3)]
            f = AL(es, "f", [P, TB, D], F32)
            xs = [AL(es, "xs%d" % i, [P, D], F32) for i in range(2)]
            gfb = AL(es, "gfb2", [P, D], F32)
            junk = AL(es, "junke", [P, D], BF16)
            S.dma(Q, gfb[:, :], gfb_sp, [], ["gfb"], "c0")
            wdv = wd_d.rearrange("(k p) n -> p k n", p=P)
            wc = 0
            for cg in range(4):
                for kg in range(11):
                    s = wc % 3
                    wc += 1
                    S.dma("pool", wd[s][:, :, :], wdv[:, kg * 4:(kg + 1) * 4, cg * 512:(cg + 1) * 512], [], [("wd", s)], ("wd", s))
                    for kk in range(4):
                        k = kg * 4 + kk
                        for tb in range(TB):
                            A("pe", mm(ps[tb][:, :], aT[:, k, tb * P:(tb + 1) * P], wd[s][:, kk, :], k == 0, k == 43), [("wd", s)], [("ps", tb)])
                for tb in range(TB):
                    if tb % 2:
                        A("act", act_fn(f[:, tb, cg * 512:(cg + 1) * 512], ps[tb][:, :], AF.Copy), [("ps", tb)], [("f", tb)])
                    else:
                        A("dve", cp(f[:, tb, cg * 512:(cg + 1) * 512], ps[tb][:, :]), [("ps", tb)], [("f", tb)])
            for tb in range(TB):
                b = tb % 2
                S.dma(Q, xs[b][:, :], x1_sp[tb * P:(tb + 1) * P, :], [], [("xs", b)], ("xs", b))
                ssq = st[:, b:b + 1]
                A("dve", lambda e, o=ssq: e.memset(o, 0.0), [], [("ssq", b)])
                A("act", act_fn(junk[:, :], f[:, tb, :], AF.Square, accum_out=ssq), [("f", tb), ("ssq", b)], ["junk", ("ssq", b)])
                r, rk = rstd_chain(ssq, ("ssq", b), D, b)
                A("dve", ts(f[:, tb, :], f[:, tb, :], r, None, ALU.mult), [("f", tb), rk], [("f", tb)])
                A("dve", tt(f[:, tb, :], f[:, tb, :], gfb[:, :], ALU.mult), [("f", tb), "gfb"], [("f", tb)])
                A("dve", tt(xs[b][:, :], xs[b][:, :], f[:, tb, :], ALU.add), [("f", tb), ("xs", b)], [("xs", b)])
                S.dma(Q, out_d[tb * P:(tb + 1) * P, :], xs[b][:, :], [("xs", b)], ["out"], ("os", b))
            fence(["out"])
            S.emit()
            if STOP == "E":
                return nc
    return nc


_CACHE = {}


def _rope_tabs(pos, dim):
    inv = (1.0 / (10000.0 ** (np.arange(0, dim, 2, dtype=np.float32) / np.float32(dim)))).astype(np.float32)
    ang = pos.astype(np.float32)[:, None] * inv[None, :]
    return np.cos(ang).astype(np.float32), np.sin(ang).astype(np.float32)


def kernel(**inp):
    f32 = np.float32
    if "nc" not in _CACHE:
        _CACHE["nc"] = build_program(DEBUG)
    nc = _CACHE["nc"]
    x = np.asarray(inp["x"], f32)[0]
    g = lambda k: np.ascontiguousarray(np.asarray(inp[k], f32)[0])
    col = lambda v, n: np.ascontiguousarray(v.reshape(n, P).T)
    shared = {
        "c_col": col(np.asarray(inp["c"], f32)[0], 16),
        "g_pre_mix_col": col(g("g_pre_mix"), 16),
        "g_pre_ffn_col": col(g("g_pre_ffn"), 16),
        "g_post_mix": g("g_post_mix")[None, :],
        "g_post_ffn": g("g_post_ffn")[None, :],
        "w_in": g("w_in"),
        "gmlp_ln_g": g("gmlp_ln_g")[None, :],
        "gmlp_ln_b": g("gmlp_ln_b")[None, :],
        "gmlp_wsT": np.ascontiguousarray(g("gmlp_w_s").transpose(2, 0, 1)),
        "gmlp_bs_col": np.ascontiguousarray(g("gmlp_b_s").T),
        "q_lat_g_col": col(g("q_lat_norm_g"), 4),
        "w_q_up": g("w_q_up"), "w_qidx_up": g("w_qidx_up"),
        "kidx_ln_g": g("kidx_ln_g")[None, :], "kidx_ln_b": g("kidx_ln_b")[None, :],
        "w_proj_a": g("w_proj_a"), "w_proj_b": g("w_proj_b"), "w_out": g("w_out"),
        "w_ffn_gate": g("w_ffn_gate"), "w_ffn_up": g("w_ffn_up"), "w_ffn_down": g("w_ffn_down"),
        "ident_bf": np.eye(P, dtype=f32).astype(ml_dtypes.bfloat16),
        "negi4": np.tile(np.eye(P, dtype=f32) * -30000.0, (1, 4)).astype(ml_dtypes.bfloat16),
        "causalT": np.triu(np.ones((P, P), f32)),
        "pow2": np.tile((0.5 ** np.arange(1, NIT + 2, dtype=np.float64)).astype(f32)[None, :], (P, 1)),
    }
    w_mod = g("w_mod")
    b_mod = g("b_mod")
    in_maps = []
    tri = np.where(np.arange(P)[None, :] <= np.arange(P)[:, None], 0.0, NEG).astype(f32)
    for c in range(NC):
        blks = core_blocks(c)
        rows = np.concatenate([np.arange(b * P, (b + 1) * P) for b in blks])
        pos = rows.reshape(TB, P)
        ca, sa = _rope_tabs(rows, 128)
        ci, si = _rope_tabs(rows, 32)
        c2 = np.concatenate([ca, ca], 1).reshape(TB, P, 128).transpose(1, 0, 2)
        s2 = np.concatenate([-sa, sa], 1).reshape(TB, P, 128).transpose(1, 0, 2)
        ci2 = np.concatenate([ci, ci], 1).reshape(TB, P, 32).transpose(1, 0, 2)
        si2 = np.concatenate([-si, si], 1).reshape(TB, P, 32).transpose(1, 0, 2)
        madd = np.zeros((TB, P, 8, P), f32)
        for lb in range(TB):
            qb = blks[lb]
            for r in range(NC):
                kb_ = core_blocks(r)[lb]
                if kb_ > qb:
                    madd[lb, :, r, :] = NEG
                elif kb_ == qb:
                    madd[lb, :, r, :] = tri
        m = dict(shared)
        m.update({
            "x": np.ascontiguousarray(x[rows]),
            "w_mod_sl": np.ascontiguousarray(w_mod[:, c * 1536:(c + 1) * 1536]),
            "b_mod_sl": np.ascontiguousarray(b_mod[None, c * 1536:(c + 1) * 1536]),
            "rope_c2": np.ascontiguousarray(c2), "rope_s2": np.ascontiguousarray(s2),
            "rope_ci2": np.ascontiguousarray(ci2), "rope_si2": np.ascontiguousarray(si2),
            "maskadd": madd,
        })
        in_maps.append(m)
    res = run_bass_kernel_spmd(nc, in_maps, core_ids=list(range(NC)))
    if DEBUG:
        _DBG["res"] = res.results
    out = np.empty((8192, D), f32)
    for c in range(NC):
        blks = core_blocks(c)
        o = np.asarray(res.results[c]["out"], f32)
        for lb, b in enumerate(blks):
            out[b * P:(b + 1) * P] = o[lb * P:(lb + 1) * P]
    return out[None]
```
